# Optimizing a Trainium2 kernel written in Bass

```python
import jax
import jax.numpy as jnp
from jax import lax
import numpy as np

D_MODEL = 2048
BATCH = 4
SEQ = 2048
DEPTH = 1

EPS = 1e-6
D_MIX = D_MODEL

HG_HEADS = 8
HG_DK = 128
HG_DV = 128
HG_WIDTH = HG_HEADS * HG_DV
HG_CHUNK = 64

MLA_HEADS = 8
MLA_Q_RANK = 512
MLA_KV_RANK = 256
MLA_NOPE = 128
MLA_ROPE = 64
MLA_QK = MLA_NOPE + MLA_ROPE
MLA_V = 128
MLA_WIDTH = MLA_HEADS * MLA_V
ATTN_BLOCK = 128
ROPE_THETA = 10000.0

IN_SIZES = (HG_HEADS * HG_DK, HG_HEADS * HG_DK, HG_HEADS * HG_DV, HG_HEADS * HG_DV,
            MLA_Q_RANK, MLA_KV_RANK, MLA_ROPE)
IN_COLS = sum(IN_SIZES)

N_EXPERTS = 64
TOP_K = 8
N_GROUPS = 8
TOPK_GROUPS = 4
EXPERTS_PER_GROUP = N_EXPERTS // N_GROUPS
D_EXPERT = 512
ROUTED_SCALE = 2.5
MOE_BLOCK = 128

kernel_name = 'hymba_hgrn2_mla_moe_adaln'


def rms_norm(x, g):
    xf = x.astype(jnp.float32)
    y = xf * lax.rsqrt(jnp.mean(xf * xf, axis=-1, keepdims=True) + EPS)
    return (y * g.astype(jnp.float32)).astype(x.dtype)


def apply_rope(x, pos):
    half = MLA_ROPE // 2
    inv_freq = ROPE_THETA ** (-jnp.arange(half, dtype=jnp.float32) / half)
    ang = pos.astype(jnp.float32)[:, :, None, None] * inv_freq
    cos, sin = jnp.cos(ang), jnp.sin(ang)
    xf = x.astype(jnp.float32)
    x1, x2 = xf[..., :half], xf[..., half:]
    return jnp.concatenate([x1 * cos - x2 * sin, x2 * cos + x1 * sin], axis=-1).astype(x.dtype)


def hgrn2_group(q, f_logit, i_in, g, lb, out_g):
    B, S, _ = q.shape
    n_chunks = S // HG_CHUNK
    f32 = jnp.float32
    f = lb + (1.0 - lb) * jax.nn.sigmoid(f_logit.astype(f32))
    log_f = jnp.log(f)
    k = 1.0 - f

    def to_chunks(t, d):
        return t.astype(f32).reshape(B, n_chunks, HG_CHUNK, HG_HEADS, d).transpose(1, 0, 3, 2, 4)

    qc, lfc, kc, vc = to_chunks(q, HG_DK), to_chunks(log_f, HG_DK), to_chunks(k, HG_DK), to_chunks(i_in, HG_DV)
    causal = jnp.tril(jnp.ones((HG_CHUNK, HG_CHUNK), dtype=bool))[:, :, None]

    def chunk_step(state, inp):
        q_c, lf_c, k_c, v_c = inp
        b = jnp.cumsum(lf_c, axis=2)
        o_inter = jnp.einsum('bhtk,bhkv->bhtv', q_c * jnp.exp(b), state)
        rel = b[:, :, :, None, :] - b[:, :, None, :, :]
        decay = jnp.exp(jnp.where(causal, rel, -jnp.inf))
        attn = jnp.einsum('bhtk,bhtsk,bhsk->bhts', q_c, decay, k_c)
        o_intra = jnp.einsum('bhts,bhsv->bhtv', attn, v_c)
        b_last = b[:, :, -1, :]
        state = (jnp.exp(b_last)[..., None] * state
                 + jnp.einsum('bhsk,bhsv->bhkv', k_c * jnp.exp(b_last[:, :, None, :] - b), v_c))
        return state, o_inter + o_intra

    s0 = jnp.zeros((B, HG_HEADS, HG_DK, HG_DV), f32)
    _, o = lax.scan(chunk_step, s0, (qc, lfc, kc, vc))
    o = o.transpose(1, 0, 3, 2, 4).reshape(B, S, HG_HEADS, HG_DV)
    gate = jax.nn.silu(g.astype(f32)).reshape(B, S, HG_HEADS, HG_DV)
    o = rms_norm(o, out_g) * gate
    return o.reshape(B, S, HG_WIDTH).astype(q.dtype)


def mla_group(c_q, c_kv, k_rope, pos, q_a_g, w_uq, kv_a_g, w_ukv, q_norm_g, k_norm_g):
    B, S, _ = c_q.shape
    q = (rms_norm(c_q, q_a_g) @ w_uq).reshape(B, S, MLA_HEADS, MLA_QK)
    kv = (rms_norm(c_kv, kv_a_g) @ w_ukv).reshape(B, S, MLA_HEADS, MLA_NOPE + MLA_V)
    k_nope, v = kv[..., :MLA_NOPE], kv[..., MLA_NOPE:]
    k = jnp.concatenate([k_nope, jnp.broadcast_to(k_rope[:, :, None, :], (B, S, MLA_HEADS, MLA_ROPE))], axis=-1)
    q = rms_norm(q, q_norm_g)
    k = rms_norm(k, k_norm_g)
    q = jnp.concatenate([q[..., :MLA_NOPE], apply_rope(q[..., MLA_NOPE:], pos)], axis=-1)
    k = jnp.concatenate([k[..., :MLA_NOPE], apply_rope(k[..., MLA_NOPE:], pos)], axis=-1)

    n_blocks = S // ATTN_BLOCK
    q_blocks = q.reshape(B, n_blocks, ATTN_BLOCK, MLA_HEADS, MLA_QK).transpose(1, 0, 2, 3, 4)
    key_idx = jnp.arange(S)
    scale = MLA_QK ** -0.5

    def attend(args):
        q_blk, blk = args
        q_idx = blk * ATTN_BLOCK + jnp.arange(ATTN_BLOCK)
        s = jnp.einsum('bqhd,bkhd->bhqk', q_blk, k, preferred_element_type=jnp.float32) * scale
        s = jnp.where(key_idx[None, :] <= q_idx[:, None], s, -jnp.inf)
        p = jax.nn.softmax(s, axis=-1).astype(v.dtype)
        return jnp.einsum('bhqk,bkhd->bqhd', p, v)

    o = lax.map(attend, (q_blocks, jnp.arange(n_blocks)))
    return o.transpose(1, 0, 2, 3, 4).reshape(B, S, MLA_WIDTH)


def moe(h, w_router, router_bias, w_gate, w_up, w_down, ws_gate, ws_up, ws_down):
    B, S, D = h.shape
    T = B * S
    xt = h.reshape(T, D)
    scores = jax.nn.sigmoid(jnp.einsum('td,de->te', xt, w_router, preferred_element_type=jnp.float32))
    sel = scores + router_bias.astype(jnp.float32)
    grp_score = lax.top_k(sel.reshape(T, N_GROUPS, EXPERTS_PER_GROUP), 2)[0].sum(-1)
    _, grp_idx = lax.top_k(grp_score, TOPK_GROUPS)
    grp_mask = jnp.any(grp_idx[:, :, None] == jnp.arange(N_GROUPS)[None, None, :], axis=1)
    sel = jnp.where(jnp.repeat(grp_mask, EXPERTS_PER_GROUP, axis=1), sel, -jnp.inf)
    _, top_idx = lax.top_k(sel, TOP_K)
    top_w = jnp.take_along_axis(scores, top_idx, axis=1)
    top_w = top_w / jnp.sum(top_w, axis=-1, keepdims=True) * ROUTED_SCALE

    A = T * TOP_K
    e_flat = top_idx.reshape(A)
    tok_flat = jnp.repeat(jnp.arange(T, dtype=jnp.int32), TOP_K)
    w_flat = top_w.reshape(A)
    order = jnp.argsort(e_flat)
    e_sorted, tok_sorted, w_sorted = e_flat[order], tok_flat[order], w_flat[order]
    counts = jnp.bincount(e_flat, length=N_EXPERTS)
    padded = (counts + MOE_BLOCK - 1) // MOE_BLOCK * MOE_BLOCK
    start = jnp.cumsum(counts) - counts
    pad_end = jnp.cumsum(padded)
    pad_start = pad_end - padded
    dest = pad_start[e_sorted] + (jnp.arange(A, dtype=jnp.int32) - start[e_sorted])
    P = A + N_EXPERTS * MOE_BLOCK
    n_blk = P // MOE_BLOCK
    row_tok = jnp.full((P,), T, dtype=jnp.int32).at[dest].set(tok_sorted)
    row_w = jnp.zeros((P,), jnp.float32).at[dest].set(w_sorted)
    blk_expert = jnp.minimum(jnp.searchsorted(pad_end, jnp.arange(n_blk) * MOE_BLOCK, side='right'), N_EXPERTS - 1)
    x_pad = jnp.concatenate([xt, jnp.zeros((1, D), xt.dtype)], axis=0)

    def block_step(acc, args):
        rows, wts, e = args
        xb = x_pad[rows]
        hid = jax.nn.silu(xb @ w_gate[e]) * (xb @ w_up[e])
        yb = (hid @ w_down[e]) * wts[:, None].astype(xb.dtype)
        return acc.at[rows].add(yb), None

    acc0 = jnp.zeros((T + 1, D), xt.dtype)
    acc, _ = lax.scan(block_step, acc0,
                      (row_tok.reshape(n_blk, MOE_BLOCK), row_w.reshape(n_blk, MOE_BLOCK), blk_expert))
    routed = acc[:T]
    shared = (jax.nn.silu(xt @ ws_gate) * (xt @ ws_up)) @ ws_down
    return (routed + shared).reshape(B, S, D)


def setup_inputs(seed: int = 0) -> dict:
    key = jax.random.key(seed)
    ks = jax.random.split(key, 27)
    f32 = jnp.float32
    nrm = lambda k, shape, s: jax.random.normal(k, shape, f32) * s
    gain = lambda k, shape: 1.0 + 0.02 * jax.random.normal(k, shape, f32)
    L = DEPTH
    return {
        'x': nrm(ks[0], (BATCH, SEQ, D_MODEL), 1.0),
        'c': nrm(ks[1], (BATCH, D_MODEL), 1.0),
        'positions': (jnp.arange(SEQ, dtype=jnp.int32)[None, :]
                      + jax.random.randint(ks[2], (BATCH, 1), 0, 4096, dtype=jnp.int32)),
        'w_ada': nrm(ks[3], (L, D_MODEL, 6 * D_MODEL), 0.5 * D_MODEL ** -0.5),
        'b_ada': nrm(ks[4], (L, 6 * D_MODEL), 0.02),
        'norm_mix_g': gain(ks[5], (L, D_MODEL)),
        'norm_ffn_g': gain(ks[6], (L, D_MODEL)),
        'w_in': nrm(ks[7], (L, D_MODEL, IN_COLS), D_MODEL ** -0.5),
        'hg_lb_logits': nrm(ks[8], (DEPTH + 1, HG_HEADS * HG_DK), 0.1),
        'hg_out_g': gain(ks[9], (L, HG_DV)),
        'mla_q_a_g': gain(ks[10], (L, MLA_Q_RANK)),
        'mla_w_uq': nrm(ks[11], (L, MLA_Q_RANK, MLA_HEADS * MLA_QK), MLA_Q_RANK ** -0.5),
        'mla_kv_a_g': gain(ks[12], (L, MLA_KV_RANK)),
        'mla_w_ukv': nrm(ks[13], (L, MLA_KV_RANK, MLA_HEADS * (MLA_NOPE + MLA_V)), MLA_KV_RANK ** -0.5),
        'mla_q_norm_g': gain(ks[14], (L, MLA_QK)),
        'mla_k_norm_g': gain(ks[15], (L, MLA_QK)),
        'mla_out_g': gain(ks[16], (L, MLA_WIDTH)),
        'w_out': nrm(ks[17], (L, D_MIX, D_MODEL), D_MIX ** -0.5),
        'w_router': nrm(ks[18], (L, D_MODEL, N_EXPERTS), D_MODEL ** -0.5),
        'router_bias': nrm(ks[19], (L, N_EXPERTS), 0.01),
        'w_gate': nrm(ks[20], (L, N_EXPERTS, D_MODEL, D_EXPERT), D_MODEL ** -0.5),
        'w_up': nrm(ks[21], (L, N_EXPERTS, D_MODEL, D_EXPERT), D_MODEL ** -0.5),
        'w_down': nrm(ks[22], (L, N_EXPERTS, D_EXPERT, D_MODEL), D_EXPERT ** -0.5),
        'ws_gate': nrm(ks[23], (L, D_MODEL, D_EXPERT), D_MODEL ** -0.5),
        'ws_up': nrm(ks[24], (L, D_MODEL, D_EXPERT), D_MODEL ** -0.5),
        'ws_down': nrm(ks[25], (L, D_EXPERT, D_MODEL), D_EXPERT ** -0.5),
    }


def reference(x, c, positions, w_ada, b_ada, norm_mix_g, norm_ffn_g, w_in, hg_lb_logits, hg_out_g,
              mla_q_a_g, mla_w_uq, mla_kv_a_g, mla_w_ukv, mla_q_norm_g, mla_k_norm_g, mla_out_g,
              w_out, w_router, router_bias, w_gate, w_up, w_down, ws_gate, ws_up, ws_down):
    lb_all = jnp.cumsum(jax.nn.softmax(hg_lb_logits.astype(jnp.float32), axis=0), axis=0)
    cond = jax.nn.silu(c)
    offs = [int(o) for o in np.cumsum(IN_SIZES)[:-1]]
    h = x
    for l in range(DEPTH):
        mod = cond @ w_ada[l] + b_ada[l]
        sh_m, sc_m, gt_m, sh_f, sc_f, gt_f = jnp.split(mod, 6, axis=-1)

        u = rms_norm(h, norm_mix_g[l]) * (1.0 + sc_m[:, None, :]) + sh_m[:, None, :]
        proj = u @ w_in[l]
        hq, hf, hi, hgate, cq, ckv, krope = jnp.split(proj, offs, axis=-1)
        o_hg = hgrn2_group(hq, hf, hi, hgate, lb_all[l], hg_out_g[l])
        o_mla = mla_group(cq, ckv, krope, positions, mla_q_a_g[l], mla_w_uq[l], mla_kv_a_g[l],
                          mla_w_ukv[l], mla_q_norm_g[l], mla_k_norm_g[l])
        merged = jnp.concatenate([o_hg, rms_norm(o_mla, mla_out_g[l])], axis=-1)
        h = h + gt_m[:, None, :] * (merged @ w_out[l])

        u = rms_norm(h, norm_ffn_g[l]) * (1.0 + sc_f[:, None, :]) + sh_f[:, None, :]
        h = h + gt_f[:, None, :] * moe(u, w_router[l], router_bias[l], w_gate[l], w_up[l], w_down[l],
                                       ws_gate[l], ws_up[l], ws_down[l])
    return h
```

```python
import math
from contextlib import ExitStack

import numpy as np
import ml_dtypes

import concourse.bass as bass
import concourse.mybir as mybir
from concourse.bass_utils import run_bass_kernel_spmd

F32 = mybir.dt.float32
BF16 = mybir.dt.bfloat16
I32 = mybir.dt.int32
AF = mybir.ActivationFunctionType
ALU = mybir.AluOpType
AX = mybir.AxisListType

D = 2048
S = 2048
NT = 16
NOWN = 8
EPS = 1e-6
IN_COLS = 4928
N_EXP = 64
import os
DBG_SUB = os.environ.get("KSUB", "")
SCHED_WINDOW = int(os.environ.get("KWIN", "16"))
SM_SHIFT = -14.0


class Buf:
    __slots__ = ("name", "w", "r", "excl")

    def __init__(self, name, excl=False):
        self.name = name
        self.w = None
        self.r = []
        self.excl = excl


class Ins:
    __slots__ = ("eng", "fn", "deps", "signal", "sigval", "is_dma", "dsem", "dval", "alldeps", "idx", "cost", "fence", "fin")

    def __init__(self, eng, fn):
        self.eng = eng
        self.fn = fn
        self.alldeps = []
        self.idx = 0
        self.cost = 0.3
        self.fence = False
        self.fin = None
        self.deps = []
        self.signal = False
        self.sigval = None
        self.is_dma = False
        self.dsem = None
        self.dval = None


class Prog:
    ENGS = ["pe", "act", "dve", "pool", "sp"]

    def __init__(self, nc, stack):
        self.nc = nc
        self.stack = stack
        self.streams = {e: [] for e in self.ENGS}
        self.dma_sems = {}
        self.nsem = 0
        self.last = {e: None for e in self.ENGS}
        self.dma_last = {}
        self.all = []
        self.segs = [0]

    def new_sem(self, name):
        self.nsem += 1
        return self.stack.enter_context(self.nc.semaphore(name))

    def _add(self, eng, fn, reads, writes, extra=(), cost=0.3):
        ins = Ins(eng, fn)
        ins.cost = cost
        deps = list(extra)
        for b in reads:
            if b.w is not None:
                deps.append(b.w)
            if b.excl:
                deps.extend(r for r in b.r if r.eng != eng)
        for b in writes:
            if b.w is not None:
                deps.append(b.w)
            deps.extend(b.r)
        seen = set()
        dd = []
        for d in deps:
            if id(d) in seen:
                continue
            seen.add(id(d))
            dd.append(d)
        ins.alldeps = dd
        for b in reads:
            b.r.append(ins)
        for b in writes:
            b.w = ins
            b.r = []
        ins.idx = len(self.all)
        self.all.append(ins)
        return ins

    def op(self, eng, fn, reads=(), writes=(), cost=0.3):
        ins = self._add(eng, fn, list(reads), list(writes), cost=cost)
        self.last[eng] = ins
        return ins

    def dma(self, eng, fn, reads=(), writes=(), sembuf=None, cost=8.0):
        ins = self._add(eng, fn, list(reads), list(writes), cost=cost)
        ins.is_dma = True
        if sembuf not in self.dma_sems:
            self.dma_sems[sembuf] = [self.new_sem("d%d" % self.nsem), 0]
        ent = self.dma_sems[sembuf]
        ent[1] += 16
        ins.dsem = ent[0]
        ins.dval = ent[1]
        self.dma_last[sembuf] = ins
        return ins

    def barrier(self, tiny):
        self.segs.append(len(self.all))
        out = {}
        for e in self.ENGS:
            ins = self._add(e, tiny[e], [], [], cost=0.1)
            ins.fence = True
            out[e] = ins
        self.segs.append(len(self.all))
        return out

    def schedule(self, window=SCHED_WINDOW, lat=0.6):
        bounds = self.segs + [len(self.all)]
        streams = {e: [] for e in self.ENGS}
        prev_last = {}
        outstanding_dma = {}
        tbase = 0.0
        for si in range(len(bounds) - 1):
            seg = self.all[bounds[si]:bounds[si + 1]]
            if not seg:
                continue
            if seg[0].fence:
                deps = list(prev_last.values()) + list(outstanding_dma.values())
                for ins in seg:
                    ins.alldeps = [d for d in deps]
                    ins.fin = tbase
                    streams[ins.eng].append(ins)
                outstanding_dma = {}
                continue
            pend = {e: [i for i in seg if i.eng == e] for e in self.ENGS}
            pos = {e: 0 for e in self.ENGS}
            done = {e: [False] * len(pend[e]) for e in self.ENGS}
            free = {e: tbase for e in self.ENGS}
            left = len(seg)
            while left:
                best = None
                for e in self.ENGS:
                    lst = pend[e]
                    p = pos[e]
                    n = len(lst)
                    cnt = 0
                    j = p
                    seen_dma = False
                    while j < n and cnt < window:
                        if not done[e][j]:
                            ins = lst[j]
                            cnt += 1
                            if ins.is_dma:
                                if seen_dma:
                                    j += 1
                                    continue
                                seen_dma = True
                            ok = True
                            rt = free[e]
                            for d in ins.alldeps:
                                if d.fin is None:
                                    ok = False
                                    break
                                t_ = d.fin + (lat if d.eng != e or d.is_dma else 0.0)
                                if t_ > rt:
                                    rt = t_
                            if ok and (best is None or rt < best[0] - 1e-9 or (abs(rt - best[0]) <= 1e-9 and ins.idx < best[3].idx)):
                                best = (rt, e, j, ins)
                        j += 1
                rt, e, j, ins = best
                done[e][j] = True
                while pos[e] < len(pend[e]) and done[e][pos[e]]:
                    pos[e] += 1
                if ins.is_dma:
                    free[e] = rt + 0.15
                    ins.fin = rt + ins.cost
                else:
                    free[e] = rt + ins.cost
                    ins.fin = free[e]
                streams[e].append(ins)
                left -= 1
            for e in self.ENGS:
                if pend[e]:
                    ld = [i for i in streams[e] if not i.is_dma]
                    if ld:
                        prev_last[e] = ld[-1]
            for i in seg:
                if i.is_dma:
                    outstanding_dma[id(i.dsem)] = i if (id(i.dsem) not in outstanding_dma or outstanding_dma[id(i.dsem)].dval < i.dval) else outstanding_dma[id(i.dsem)]
            tbase = max(i.fin for i in seg)
        self.streams = streams
        self.sim_time = tbase

    def finalize(self):
        nc = self.nc
        LIMIT = 1 << 30
        self.schedule()
        for ins in self.all:
            dd = []
            for d in ins.alldeps:
                if d.eng == "pe" and ins.eng == "pe" and not d.is_dma:
                    continue
                dd.append(d)
                d.signal = True
            ins.deps = dd
        for e in self.ENGS:
            cnt = 0
            sem = None
            for ins in self.streams[e]:
                if ins.is_dma:
                    continue
                if ins.signal:
                    if sem is None or cnt >= LIMIT:
                        sem = self.new_sem("s%s%d" % (e, self.nsem))
                        cnt = 0
                    cnt += 1
                    ins.sigval = (sem, cnt)
        with nc.Block() as block:
            def make(e):
                def body(h):
                    waited = {}
                    for ins in self.streams[e]:
                        for d in ins.deps:
                            sem, val = (d.dsem, d.dval) if d.is_dma else d.sigval
                            if waited.get(id(sem), 0) >= val:
                                continue
                            h.wait_ge(sem, val)
                            waited[id(sem)] = val
                        r = ins.fn(h)
                        if ins.is_dma:
                            r.then_inc(ins.dsem, 16)
                        elif ins.signal:
                            r.then_inc(ins.sigval[0], 1)
                return body
            block.tensor(make("pe"))
            block.scalar(make("act"))
            block.vector(make("dve"))
            block.gpsimd(make("pool"))
            block.sync(make("sp"))


def build_program(stage=99, n_exp=N_EXP):
    nc = bass.Bass("TRN2", target_bir_lowering=False)

    def din(name, shape, dt=F32):
        return nc.dram_tensor(name, list(shape), dt, kind="ExternalInput").ap()

    xp_d = din("xp", [1024, D])
    xo_d = din("xo", [1024, D])
    vecA_d = din("vecA", [128, 128])
    vecB_d = din("vecB", [96, 128])
    pos_d = din("pos", [128, NT], F32 if False else I32)
    pm_d = din("pmask", [128, 1])
    w_ada_d = din("w_ada", [D, 6 * D])
    w_in_d = din("w_in", [D, IN_COLS])
    lbl_d = din("hg_lb_logits", [2, 1024])
    outg_d = din("hg_out_g", [128])
    w_uq_d = din("mla_w_uq", [512, 1536])
    w_ukv_d = din("mla_w_ukv", [256, 2048])
    qng_d = din("mla_q_norm_g", [192])
    kng_d = din("mla_k_norm_g", [192])
    mog_d = din("mla_out_g", [1024])
    w_out_d = din("w_out", [D, D])
    if stage >= 4:
        w_r_d = din("w_router", [D, 64])
        rb_d = din("router_bias", [64])
        w_g_d = din("w_gate", [N_EXP, D, 512])
        w_u_d = din("w_up", [N_EXP, D, 512])
        w_d_d = din("w_down", [N_EXP, 512, D])
        ws_g_d = din("ws_gate", [D, 512])
        ws_u_d = din("ws_up", [D, 512])
        ws_d_d = din("ws_down", [512, D])
    out_d = nc.dram_tensor("out", [1024, D], F32, kind="ExternalOutput").ap()
    dbg_d = None
    if stage < 4:
        dbg_d = nc.dram_tensor("dbg", [1024, D], F32, kind="ExternalOutput").ap()

    with ExitStack() as st:
        P = Prog(nc, st)

        def sb(stack, name, shape, dt):
            return stack.enter_context(nc.sbuf_tensor(name, list(shape), dt))

        PS = [st.enter_context(nc.psum_tensor("ps%d" % i, [128, 512], F32)) for i in range(8)]
        PB = [Buf("pb%d" % i, excl=True) for i in range(8)]

        def psbf(i):
            return PS[i][:, :].bitcast(BF16)

        ones_f = sb(st, "ones_f", [128, 128], F32); B_ones = Buf("ones")
        ident_f = sb(st, "ident_f", [128, 128], F32); B_identf = Buf("identf")
        ident_b = sb(st, "ident_b", [128, 128], BF16); B_identb = Buf("identb")
        tri_f = sb(st, "tri_f", [128, 128], F32); B_trif = Buf("trif")
        trs_f = sb(st, "trs_f", [128, 128], F32); B_trsf = Buf("trsf")
        tri_b = sb(st, "tri_b", [128, 128], BF16); B_trib = Buf("trib")
        tiny_t = sb(st, "tiny_t", [128, 8], F32)
        colA = sb(st, "colA", [128, 128], F32); B_colA = Buf("colA")
        colB = sb(st, "colB", [128, 96], F32); B_colB = Buf("colB")
        modT = sb(st, "modT", [128, 96], F32); B_mod1 = Buf("mod1"); B_mod2 = Buf("mod2")
        am = sb(st, "am", [128, 16], F32); B_am = Buf("am")
        af_ = sb(st, "af", [128, 16], F32); B_af = Buf("af")
        condT = sb(st, "condT", [128, 16], BF16); B_cond = Buf("cond")
        pm = sb(st, "pm", [128, 1], F32); B_pm = Buf("pm")
        posf = sb(st, "posf", [128, NT], F32); B_posf = Buf("posf")
        cosT = sb(st, "cosT", [128, NT, 32], F32); B_cos = Buf("cos")
        sinT = sb(st, "sinT", [128, NT, 32], F32); B_sin = Buf("sin")

        def tiny_fns():
            return {
                "pe": lambda h: h.matmul(PS[7][0:8, 0:8], lhsT=ident_b[:, 0:8], rhs=ident_b[:, 0:8], start=True, stop=True),
                "act": lambda h: h.copy(out=tiny_t[:, 0:1], in_=tiny_t[:, 1:2]),
                "dve": lambda h: h.tensor_copy(out=tiny_t[:, 2:3], in_=tiny_t[:, 3:4]),
                "pool": lambda h: h.tensor_copy(out=tiny_t[:, 4:5], in_=tiny_t[:, 5:6]),
                "sp": lambda h: h.nop(),
            }

        def barrier():
            fz = P.barrier(tiny_fns())
            for b in PB:
                b.w = None
                b.r = []
            PB[7].w = fz["pe"]

        def fsz(ap):
            n = 1
            for d in ap.shape[1:]:
                n *= int(d)
            return n

        def ecost(eng, ap):
            n = fsz(ap)
            if eng == "pool":
                return 0.25 + n / 500.0
            if eng == "act":
                return 0.2 + n / 1100.0
            return 0.12 + n / 950.0

        def mm(out, lhsT, rhs, start, stop, reads, writes, skip=False):
            return P.op("pe", lambda h: h.matmul(out, lhsT=lhsT, rhs=rhs, start=start, stop=stop,
                                                 skip_group_check=skip), reads, writes, cost=0.06 + fsz(rhs) / 2300.0)

        def tr(out, in_, ident, reads, writes):
            return P.op("pe", lambda h: h.transpose(out, in_, ident), reads, writes, cost=0.12)

        def act(out, in_, func, reads, writes, scale=None, bias=None, accum=None):
            kw = {}
            if scale is not None:
                kw["scale"] = scale
            if bias is not None:
                kw["bias"] = bias
            if accum is not None:
                kw["accum_out"] = accum
            return P.op("act", lambda h: h.activation(out=out, in_=in_, func=func, **kw), reads, writes, cost=ecost("act", out))

        def tt(eng, out, in0, in1, op, reads, writes):
            return P.op(eng, lambda h: h.tensor_tensor(out=out, in0=in0, in1=in1, op=op), reads, writes, cost=ecost(eng, out))

        def ts(eng, out, in0, s1, s2, op0, op1, reads, writes, accum=None):
            c = ecost(eng, out)
            if op1 is None:
                return P.op(eng, lambda h: h.tensor_scalar(out=out, in0=in0, scalar1=s1, scalar2=None, op0=op0), reads, writes, cost=c)
            if accum is not None:
                return P.op(eng, lambda h: h.tensor_scalar(out=out, in0=in0, scalar1=s1, scalar2=s2, op0=op0, op1=op1, accum_out=accum), reads, writes, cost=c)
            return P.op(eng, lambda h: h.tensor_scalar(out=out, in0=in0, scalar1=s1, scalar2=s2, op0=op0, op1=op1), reads, writes, cost=c)

        def stt(out, in0, scalar, in1, op0, op1, reads, writes):
            return P.op("dve", lambda h: h.scalar_tensor_tensor(out=out, in0=in0, scalar=scalar, in1=in1, op0=op0, op1=op1), reads, writes,
                        cost=ecost("dve", out))

        def cp(eng, out, in_, reads, writes):
            if eng == "act":
                return P.op("act", lambda h: h.copy(out=out, in_=in_), reads, writes, cost=ecost("act", out))
            return P.op(eng, lambda h: h.tensor_copy(out=out, in_=in_), reads, writes, cost=ecost(eng, out))

        def memset(eng, ap, val, writes):
            return P.op(eng, lambda h: h.memset(ap, val), [], writes, cost=ecost(eng, ap))

        def load(eng, out, in_, writes, sembuf, reads=()):
            return P.dma(eng, lambda h: h.dma_start(out=out, in_=in_), reads, writes, sembuf=sembuf, cost=2.0 + fsz(out) * 128 * 4 / 300e3)

        def rstd_from_ss(out, ss, n, reads, writes, tmp, Btmp):
            act(tmp, ss, AF.Ln, reads, [Btmp], scale=1.0 / n, bias=eps_t[:, 0:1])
            act(out, tmp, AF.Exp, [Btmp], writes, scale=-0.5)

        eps_t = sb(st, "eps_t", [128, 1], F32); B_eps = Buf("eps")
        one_t = sb(st, "one_t", [128, 1], F32)
        shift_t = sb(st, "shift_t", [128, 1], F32)
        memset("pool", tiny_t[:, :], 0.0, [])
        memset("pool", eps_t[:, :], EPS, [B_eps])
        memset("pool", one_t[:, :], 1.0, [B_eps])
        memset("pool", shift_t[:, :], SM_SHIFT, [B_eps])
        memset("pool", ones_f[:, :], 1.0, [B_ones])
        P.op("pool", lambda h: h.affine_select(out=ident_f[:, :], in_=ones_f[:, :], pattern=[[1, 128]], compare_op=ALU.is_equal,
                                               fill=0.0, base=0, channel_multiplier=-1), [B_ones], [B_identf])
        P.op("pool", lambda h: h.affine_select(out=tri_f[:, :], in_=ones_f[:, :], pattern=[[1, 128]], compare_op=ALU.is_ge,
                                               fill=0.0, base=0, channel_multiplier=-1), [B_ones], [B_trif])
        P.op("pool", lambda h: h.affine_select(out=trs_f[:, :], in_=ones_f[:, :], pattern=[[-1, 128]], compare_op=ALU.is_gt,
                                               fill=0.0, base=0, channel_multiplier=1), [B_ones], [B_trsf])
        cp("pool", ident_b[:, :], ident_f[:, :], [B_identf], [B_identb])
        cp("pool", tri_b[:, :], tri_f[:, :], [B_trif], [B_trib])
        load("sp", pm[:, :], pm_d[:, :], [B_pm], B_pm)

        merged = sb(st, "merged", [128, NOWN, D], BF16)
        B_mg = [Buf("mg%d" % t) for t in range(NOWN)]
        ARENA = [sb(st, "arena%d" % i, [128, 8192], BF16) for i in range(2)]
        B_AR = [Buf("ar0"), Buf("ar1")]
        mx = ExitStack()
        st.enter_context(mx)
        uT = sb(mx, "uT", [128, 16, NT * 128], BF16)
        B_uT = [Buf("uT%d" % t) for t in range(NT)]

        ph1 = ExitStack()
        st.enter_context(ph1)
        if True:
            ph = ph1
            vA = sb(ph, "vA", [128, 128], F32); B_vA = Buf("vA")
            vB = sb(ph, "vB", [96, 128], F32); B_vB = Buf("vB")
            posi = sb(ph, "posi", [128, NT], I32); B_posi = Buf("posi")
            load("sp", vA[:, :], vecA_d[:, :], [B_vA], B_vA)
            load("sp", vB[:, :], vecB_d[:, :], [B_vB], B_vB)
            load("sp", posi[:, :], pos_d[:, :], [B_posi], B_posi)
            tr(PS[0][:, 0:128], vA[:, :], ident_f[:, :], [B_vA, B_identf], [PB[0]])
            cp("dve", colA[:, :], PS[0][:, 0:128], [PB[0]], [B_colA])
            tr(PS[1][:, 0:96], vB[:, :], ident_f[0:96, 0:96], [B_vB, B_identf], [PB[1]])
            cp("dve", colB[:, :], PS[1][:, 0:96], [PB[1]], [B_colB])
            cp("dve", posf[:, :], posi[:, :], [B_posi], [B_posf])
            ctmp = sb(ph, "ctmp", [128, 16], F32); B_ctmp = Buf("ctmp")
            act(ctmp[:, :], colA[:, 0:16], AF.Exp, [B_colA], [B_ctmp], scale=-1.0)
            ts("dve", ctmp[:, :], ctmp[:, :], 1.0, None, ALU.add, None, [B_ctmp], [B_ctmp])
            P.op("dve", lambda h: h.reciprocal(out=ctmp[:, :], in_=ctmp[:, :]), [B_ctmp], [B_ctmp])
            tt("dve", condT[:, :], ctmp[:, :], colA[:, 0:16], ALU.mult, [B_ctmp, B_colA], [B_cond])

            jf = sb(ph, "jf", [128, 32], F32); B_jf = Buf("jf")
            ji = sb(ph, "ji", [128, 32], I32); B_ji = Buf("ji")
            P.op("pool", lambda h: h.iota(ji[:, :], pattern=[[1, 32]], base=0, channel_multiplier=0), [], [B_ji])
            cp("dve", jf[:, :], ji[:, :], [B_ji], [B_jf])
            invf = sb(ph, "invf", [128, 32], F32); B_invf = Buf("invf")
            l2p = sb(ph, "l2p", [128, 1], F32)
            memset("pool", l2p[:, :], -math.log(2.0 * math.pi), [B_invf])
            act(invf[:, :], jf[:, :], AF.Exp, [B_jf, B_invf], [B_invf], scale=-math.log(10000.0) / 32.0, bias=l2p[:, 0:1])
            turn = sb(ph, "turn", [128, NT, 32], F32); B_turn = Buf("turn")
            turi = sb(ph, "turi", [128, NT, 32], I32); B_turi = Buf("turi")
            turf = sb(ph, "turf", [128, NT, 32], F32); B_turf = Buf("turf")
            msk = sb(ph, "msk", [128, NT, 32], F32); B_msk = Buf("msk")
            for t in range(NT):
                ts("dve", turn[:, t, :], invf[:, :], posf[:, t:t + 1], None, ALU.mult, None, [B_invf, B_posf], [B_turn])
            for which, dst, B_dst in ((0, sinT, B_sin), (1, cosT, B_cos)):
                if which == 1:
                    ts("dve", turn[:, :, :], turn[:, :, :], 0.25, None, ALU.add, None, [B_turn], [B_turn])
                cp("dve", turi[:, :, :], turn[:, :, :], [B_turn], [B_turi])
                cp("dve", turf[:, :, :], turi[:, :, :], [B_turi], [B_turf])
                tt("dve", turf[:, :, :], turn[:, :, :], turf[:, :, :], ALU.subtract, [B_turn, B_turf], [B_turf])
                ts("dve", msk[:, :, :], turf[:, :, :], 0.5, None, ALU.is_gt, None, [B_turf], [B_msk])
                tt("dve", turf[:, :, :], turf[:, :, :], msk[:, :, :], ALU.subtract, [B_turf, B_msk], [B_turf])
                ts("dve", msk[:, :, :], turf[:, :, :], -0.5, None, ALU.is_lt, None, [B_turf], [B_msk])
                tt("dve", turf[:, :, :], turf[:, :, :], msk[:, :, :], ALU.add, [B_turf, B_msk], [B_turf])
                act(dst[:, :, :], turf[:, :, :], AF.Sin, [B_turf], [B_dst], scale=2.0 * math.pi)

            WA = [ARENA[i][:, :].rearrange("p (k c) -> p k c", k=16) for i in range(2)]
            B_WA = B_AR
            wa_v = w_ada_d.rearrange("(k p) c -> p k c", p=128)

            def ada_group(g):
                wb = WA[g % 2]
                bw = B_WA[g % 2]
                P.dma("pool", lambda hh: hh.dma_start(out=wb, in_=wa_v[:, :, g * 512:(g + 1) * 512]), [], [bw], sembuf=bw, cost=16.0)
                bank = 2 if g < 8 else 3
                for jj in range(4):
                    j = g * 4 + jj
                    for k in range(16):
                        mm(PS[bank][:, j:j + 1], wb[:, k, jj * 128:(jj + 1) * 128], condT[:, k:k + 1], k == 0, k == 15,
                           [bw, B_cond], [PB[bank]])

            for g in range(8):
                ada_group(g)
            tt("dve", modT[:, 0:32], PS[2][:, 0:32], colB[:, 0:32], ALU.add, [PB[2], B_colB], [B_mod1])
            ts("dve", am[:, :], modT[:, 16:32], 1.0, None, ALU.add, None, [B_mod1], [B_am])
            tt("dve", am[:, :], am[:, :], colA[:, 16:32], ALU.mult, [B_am, B_colA], [B_am])

            def ada_finish():
                tt("dve", modT[:, 32:96], PS[3][:, 32:96], colB[:, 32:96], ALU.add, [PB[3], B_colB], [B_mod2])
                ts("dve", af_[:, :], modT[:, 64:80], 1.0, None, ALU.add, None, [B_mod2], [B_af])
                tt("dve", af_[:, :], af_[:, :], colA[:, 32:48], ALU.mult, [B_af, B_colA], [B_af])

        def norm_to_T(ph, src_fn, ntiles, dstT, B_dst, a_col, s_col, B_a, B_s, tag, src_reads=None, after_tile=None):
            yb = [sb(ph, tag + "yb%d" % i, [128, D], BF16) for i in range(2)]
            B_yb = [Buf("yb0"), Buf("yb1")]
            ssq = sb(ph, tag + "ssq", [128, ntiles], F32)
            rsd = sb(ph, tag + "rsd", [128, ntiles], F32)
            lnt = sb(ph, tag + "lnt", [128, ntiles], F32)
            for t in range(ntiles):
                xa, B_x = src_fn(t)
                y = yb[t % 2]
                By = B_yb[t % 2]
                B_ss = Buf("ss")
                B_rs = Buf("rs")
                B_ln = Buf("ln")
                act(y[:, :], xa, AF.Square, [B_x], [By, B_ss], accum=ssq[:, t:t + 1])
                rstd_from_ss(rsd[:, t:t + 1], ssq[:, t:t + 1], float(D), [B_ss, B_eps], [B_rs], lnt[:, t:t + 1], B_ln)
                ts("dve", y[:, :], xa, rsd[:, t:t + 1], None, ALU.mult, None, [B_x, B_rs, By], [By])
                for half in range(2):
                    bank = 4 + (2 * t + half) % 4
                    pv = psbf(bank)
                    for c8 in range(8):
                        c = half * 8 + c8
                        tr(pv[:, c8 * 128:(c8 + 1) * 128], y[:, c * 128:(c + 1) * 128], ident_b[:, :], [By, B_identb], [PB[bank]])
                    if half == 0:
                        B_half = Buf("evA")
                    for c8 in range(8):
                        c = half * 8 + c8
                        o = dstT[:, c, t * 128:(t + 1) * 128]
                        i_ = pv[:, c8 * 128:(c8 + 1) * 128]
                        if half == 0:
                            act(o, i_, AF.Identity, [PB[bank], B_a, B_s], [B_half], scale=a_col[:, c:c + 1], bias=s_col[:, c:c + 1])
                        elif c8 < 7:
                            ts("dve", o, i_, a_col[:, c:c + 1], s_col[:, c:c + 1], ALU.mult, ALU.add, [PB[bank], B_a, B_s], [B_dst[t]])
                        else:
                            ts("dve", o, i_, a_col[:, c:c + 1], s_col[:, c:c + 1], ALU.mult, ALU.add, [PB[bank], B_a, B_s, B_half], [B_dst[t]])
                if after_tile is not None:
                    after_tile(t)

        with ExitStack() as ph:
            xt = [sb(ph, "xt%d" % i, [128, D], F32) for i in range(2)]
            B_xt = [Buf("xt0"), Buf("xt1")]

            def src_fn(t):
                src = xp_d if t < 8 else xo_d
                r0 = (t % 8) * 128
                load("sp", xt[t % 2][:, :], src[r0:r0 + 128, :], [B_xt[t % 2]], B_xt[t % 2])
                return xt[t % 2][:, :], B_xt[t % 2]

            norm_to_T(ph, src_fn, NT, uT, B_uT, am, modT, B_am, B_mod1, "n1", after_tile=lambda t: ada_group(8 + t))
            ada_finish()
            barrier()
        ph1.close()

        with ExitStack() as ph:
            omlb = sb(ph, "omlb", [128, 1024], F32); B_omlb = Buf("omlb")
            l1t = sb(ph, "l1t", [128, 1024], F32); B_l1t = Buf("l1t")
            outg = sb(ph, "outg", [128, 128], F32); B_outg = Buf("outg")
            load("sp", omlb[:, :], lbl_d[0, :].partition_broadcast(128), [B_omlb], B_omlb)
            load("sp", l1t[:, :], lbl_d[1, :].partition_broadcast(128), [B_l1t], B_l1t)
            load("sp", outg[:, :], outg_d.partition_broadcast(128), [B_outg], B_outg)
            tt("dve", omlb[:, :], omlb[:, :], l1t[:, :], ALU.subtract, [B_omlb, B_l1t], [B_omlb])
            act(omlb[:, :], omlb[:, :], AF.Exp, [B_omlb], [B_omlb])
            ts("dve", omlb[:, :], omlb[:, :], 1.0, None, ALU.add, None, [B_omlb], [B_omlb])
            P.op("dve", lambda h: h.reciprocal(out=omlb[:, :], in_=omlb[:, :]), [B_omlb], [B_omlb])

            hqT = sb(ph, "hqT", [128, 1024], F32); B_hqT = [Buf("hqT0"), Buf("hqT1")]
            kt = sb(ph, "kt", [128, NT, 128], F32); B_kt = [Buf("kt%d" % t) for t in range(NT)]
            lf = sb(ph, "lf", [128, NT, 128], F32); B_lf = Buf("lf")
            vv = sb(ph, "vv", [128, NT, 128], BF16); B_vv = [Buf("vv%d" % t) for t in range(NT)]
            sg = sb(ph, "sg", [128, NOWN, 128], F32); B_sg = [Buf("sg%d" % t) for t in range(NOWN)]
            sgm = [sb(ph, "sgm%d" % i, [128, 128], F32) for i in range(2)]; B_sgm = [Buf("sgm0"), Buf("sgm1")]
            S_f = sb(ph, "S_f", [128, 128], F32); B_S = Buf("S")
            S_b = sb(ph, "S_b", [128, 128], BF16); B_Sb = Buf("Sb")
            R = 3
            ec = [sb(ph, "ec%d" % i, [128, 128], F32) for i in range(R)]; B_ec = [Buf("ec%d" % i) for i in range(R)]
            eb = [sb(ph, "eb%d" % i, [128, 128], F32) for i in range(R)]; B_eb = [Buf("eb%d" % i) for i in range(R)]
            enb = [sb(ph, "enb%d" % i, [128, 128], F32) for i in range(R)]; B_enb = [Buf("enb%d" % i) for i in range(R)]
            dec = [sb(ph, "dec%d" % i, [128, 1], F32) for i in range(R)]; B_dec = [Buf("dec%d" % i) for i in range(R)]
            Kh = [sb(ph, "Kh%d" % i, [128, 128], BF16) for i in range(R)]; B_Kh = [Buf("Kh%d" % i) for i in range(R)]
            Qt = [sb(ph, "Qt%d" % i, [128, 128], BF16) for i in range(R)]; B_Qt = [Buf("Qt%d" % i) for i in range(R)]
            Kt = [sb(ph, "Kt%d" % i, [128, 128], BF16) for i in range(R)]; B_Kt = [Buf("Kt%d" % i) for i in range(R)]
            ATm = [sb(ph, "ATm%d" % i, [128, 128], BF16) for i in range(R)]; B_ATm = [Buf("ATm%d" % i) for i in range(R)]
            osq = sb(ph, "osq", [128, 128], F32); B_osq = Buf("osq")
            oss = sb(ph, "oss", [128, 4], F32)
            B_o1 = Buf("o1"); B_o2 = Buf("o2"); B_o3 = Buf("o3")

            w4 = w_in_d[:, 0:4096].rearrange("(k p) (j x) -> p k j x", p=128, x=1024)

            w3 = w_in_d.rearrange("(k p) c -> p k c", p=128)

            def load_head_w(h):
                dst = ARENA[h % 2][:, :].rearrange("p (k c) -> p k c", k=16)
                for j in range(4):
                    c0 = j * 1024 + h * 128
                    P.dma("pool", (lambda j=j, c0=c0: (lambda hh: hh.dma_start(out=dst[:, :, j * 128:(j + 1) * 128],
                                                                              in_=w3[:, :, c0:c0 + 128])))(),
                          [], [B_AR[h % 2]], sembuf=B_AR[h % 2])

            load_head_w(0)
            for h in range(8):
                wa = ARENA[h % 2][:, :].rearrange("p (k c) -> p k c", k=16)
                bw = B_AR[h % 2]
                if h + 1 < 8:
                    load_head_w(h + 1)
                for half in range(2):
                    tiles = [8 + half * 4 + i for i in range(4)]
                    for k in range(16):
                        mm(PS[2][:, :], wa[:, k, 0:128], uT[:, k, 1024 + half * 512:1024 + (half + 1) * 512], k == 0, k == 15,
                           [bw] + [B_uT[t] for t in tiles], [PB[2]])
                    cp("act", hqT[:, half * 512:(half + 1) * 512], PS[2][:, :], [PB[2]], [B_hqT[half]])
                for t in range(NT):
                    own = t >= 8
                    ncol = 384 if own else 256
                    bank = t % 2
                    for k in range(16):
                        mm(PS[bank][:, 0:ncol], uT[:, k, t * 128:(t + 1) * 128], wa[:, k, 128:128 + ncol], k == 0, k == 15,
                           [bw, B_uT[t]], [PB[bank]])
                    act(kt[:, t, :], PS[bank][:, 0:128], AF.Sigmoid, [PB[bank]], [B_kt[t]], scale=-1.0)
                    tt("pool", kt[:, t, :], kt[:, t, :], omlb[:, h * 128:(h + 1) * 128], ALU.mult, [B_kt[t], B_omlb], [B_kt[t]])
                    if own:
                        cp("act", vv[:, t, :], PS[bank][:, 128:256], [PB[bank]], [B_vv[t]])
                        i2 = t % 2
                        act(sgm[i2][:, :], PS[bank][:, 256:384], AF.Sigmoid, [PB[bank]], [B_sgm[i2]])
                        tt("dve", sg[:, t - 8, :], sgm[i2][:, :], PS[bank][:, 256:384], ALU.mult, [B_sgm[i2], PB[bank]], [B_sg[t - 8]])
                        tt("pool", sg[:, t - 8, :], sg[:, t - 8, :], outg[:, :], ALU.mult, [B_sg[t - 8], B_outg], [B_sg[t - 8]])
                    else:
                        act(vv[:, t, :], PS[bank][:, 128:256], AF.Identity, [PB[bank], B_pm], [B_vv[t]], scale=pm[:, 0:1])
                act(lf[:, :, :], kt[:, :, :], AF.Ln, B_kt + [B_eps], [B_lf], scale=-1.0, bias=one_t[:, 0:1])
                memset("pool", S_f[:, :], 0.0, [B_S])
                memset("pool", S_b[:, :], 0.0, [B_Sb])

                def cum(t):
                    bank = 3 + t % 2
                    own = t >= 8
                    mm(PS[bank][:, 0:128], lf[:, t, :], tri_f[:, :], True, True, [B_lf, B_trif], [PB[bank]])
                    mm(PS[bank][:, 128:256], trs_f[:, :], lf[:, t, :], True, True, [B_lf, B_trsf], [PB[bank]])
                    if own:
                        tr(PS[bank][:, 256:384], kt[:, t, :], ident_f[:, :], [B_kt[t], B_identf], [PB[bank]])
                    i = t % R
                    act(ec[i][:, :], PS[bank][:, 128:256], AF.Exp, [PB[bank]], [B_ec[i]])
                    act(dec[i][:, :], PS[bank][:, 127:128], AF.Exp, [PB[bank]], [B_dec[i]])
                    tt("pool", Kh[i][:, :], kt[:, t, :], ec[i][:, :], ALU.mult, [B_kt[t], B_ec[i]], [B_Kh[i]])
                    if own:
                        act(eb[i][:, :], PS[bank][:, 0:128], AF.Exp, [PB[bank]], [B_eb[i]])
                        act(enb[i][:, :], PS[bank][:, 0:128], AF.Exp, [PB[bank]], [B_enb[i]], scale=-1.0)
                        c0 = (t - 8) * 128
                        tt("pool", Qt[i][:, :], hqT[:, c0:c0 + 128], eb[i][:, :], ALU.mult, [B_hqT[(t - 8) // 4], B_eb[i]], [B_Qt[i]])
                        tt("dve", Kt[i][:, :], PS[bank][:, 256:384], enb[i][:, :], ALU.mult, [PB[bank], B_enb[i]], [B_Kt[i]])

                cum(0)
                for t in range(NT):
                    own = t >= 8
                    i = t % R
                    if t + 1 < NT:
                        cum(t + 1)
                    if own:
                        mm(PS[5][:, 0:128], Kt[i][:, :], Qt[i][:, :], True, True, [B_Kt[i], B_Qt[i]], [PB[5]])
                        tt("dve", ATm[i][:, :], PS[5][:, 0:128], tri_f[:, :], ALU.mult, [PB[5], B_trif], [B_ATm[i]])
                        mm(PS[6][:, 0:128], Qt[i][:, :], S_b[:, :], True, False, [B_Qt[i], B_Sb], [PB[6]])
                        mm(PS[6][:, 0:128], ATm[i][:, :], vv[:, t, :], False, True, [B_ATm[i], B_vv[t]], [PB[6]])
                    mm(PS[7][:, 0:128], Kh[i][:, :], vv[:, t, :], True, True, [B_Kh[i], B_vv[t]], [PB[7]])
                    stt(S_f[:, :], S_f[:, :], dec[i][:, 0:1], PS[7][:, 0:128], ALU.mult, ALU.add, [B_S, B_dec[i], PB[7]], [B_S])
                    cp("act", S_b[:, :], S_f[:, :], [B_S], [B_Sb])
                    if own:
                        act(osq[:, :], PS[6][:, 0:128], AF.Square, [PB[6]], [B_osq, B_o1], accum=oss[:, 0:1])
                        rstd_from_ss(oss[:, 2:3], oss[:, 0:1], 128.0, [B_o1, B_eps], [B_o3], oss[:, 1:2], B_o2)
                        stt(merged[:, t - 8, h * 128:(h + 1) * 128], PS[6][:, 0:128], oss[:, 2:3], sg[:, t - 8, :], ALU.mult, ALU.mult,
                            [PB[6], B_o3, B_sg[t - 8]], [B_mg[t - 8]])
            barrier()

        if stage == 1:
            with ExitStack() as ph:
                dmp = sb(ph, "dmp", [128, NOWN, 1024], F32); B_dmp = Buf("dmp")
                for t in range(NOWN):
                    cp("dve", dmp[:, t, :], merged[:, t, 0:1024], [B_mg[t], B_dmp], [B_dmp])
                load("sp", dbg_d.rearrange("(t p) d -> p t d", p=128)[:, :, 0:1024], dmp[:, :, :], [], B_dmp, reads=[B_dmp])
                P.op("sp", lambda h: h.nop(), [], [B_dmp])
                P.finalize()
            return nc

        SCALE = 192.0 ** -0.5
        with ExitStack() as ph:
            cqnT = sb(ph, "cqnT", [128, 4, 1024], BF16); B_cqnT = [Buf("cqnT%d" % t) for t in range(NOWN)]
            ckvnT = sb(ph, "ckvnT", [128, 2, NT * 128], BF16); B_ckvnT = [Buf("ckvnT%d" % t) for t in range(NT)]
            KRg = sb(ph, "KRg", [128, NT, 64], F32); B_KRg = [Buf("KRg%d" % t) for t in range(NT)]
            ssr = sb(ph, "ssr", [128, NT], F32); B_ssr = [Buf("ssr%d" % t) for t in range(NT)]
            QNGr = sb(ph, "QNGr", [128, 64], F32); B_QNGr = Buf("QNGr")
            KNGr = sb(ph, "KNGr", [128, 64], F32); B_KNGr = Buf("KNGr")
            MOG = sb(ph, "MOG", [128, 1024], F32); B_MOG = Buf("MOG")
            load("sp", QNGr[:, :], qng_d[128:192].partition_broadcast(128), [B_QNGr], B_QNGr)
            load("sp", KNGr[:, :], kng_d[128:192].partition_broadcast(128), [B_KNGr], B_KNGr)
            load("sp", MOG[:, :], mog_d.partition_broadcast(128), [B_MOG], B_MOG)
            sm = sb(ph, "sm", [128, 64], F32)
            B_sm = [Buf("sm%d" % i) for i in range(64)]
            ckvn = [sb(ph, "ckvn%d" % i, [128, 256], BF16) for i in range(2)]; B_ckvn = [Buf("ckvn0"), Buf("ckvn1")]
            cqn = [sb(ph, "cqn%d" % i, [128, 512], BF16) for i in range(2)]; B_cqn = [Buf("cqn0"), Buf("cqn1")]
            junk = sb(ph, "junk", [128, 512], BF16); B_junk = Buf("junk")
            xg = [sb(ph, "xg%d" % i, [128, 64], F32) for i in range(2)]; B_xg = [Buf("xg0"), Buf("xg1")]
            rt = [sb(ph, "rt%d" % i, [128, 4, 32], F32) for i in range(2)]; B_rt = [Buf("rt0"), Buf("rt1")]

            def rope(eng, dst1, dst2, x, cosv, sinv, tmp, reads, B_tmp, writes):
                tt(eng, tmp[:, 0, :], x[:, 0:32], cosv, ALU.mult, reads, [B_tmp])
                tt(eng, tmp[:, 1, :], x[:, 32:64], sinv, ALU.mult, reads, [B_tmp])
                tt(eng, tmp[:, 2, :], x[:, 32:64], cosv, ALU.mult, reads, [B_tmp])
                tt(eng, tmp[:, 3, :], x[:, 0:32], sinv, ALU.mult, reads, [B_tmp])
                tt(eng, dst1, tmp[:, 0, :], tmp[:, 1, :], ALU.subtract, [B_tmp], writes)
                tt(eng, dst2, tmp[:, 2, :], tmp[:, 3, :], ALU.add, [B_tmp], writes)

            A0 = ARENA[0][:, :].rearrange("p (k c) -> p k c", k=16)
            A1 = ARENA[1][:, 0:16 * 320].rearrange("p (k c) -> p k c", k=16)
            P.dma("pool", lambda hh: hh.dma_start(out=A0, in_=w3[:, :, 4096:4608]), [], [B_AR[0]], sembuf=B_AR[0])
            P.dma("pool", lambda hh: hh.dma_start(out=A1, in_=w3[:, :, 4608:4928]), [], [B_AR[1]], sembuf=B_AR[1])
            for t in range(NT):
                own = t >= 8
                i2 = t % 2
                bk = 2 + i2
                for k in range(16):
                    mm(PS[bk][:, 0:320], uT[:, k, t * 128:(t + 1) * 128], A1[:, k, :], k == 0, k == 15, [B_AR[1], B_uT[t]], [PB[bk]])
                s0, s1, s2 = 0 + 8 * i2, 1 + 8 * i2, 2 + 8 * i2
                act(junk[:, 0:256], PS[bk][:, 0:256], AF.Square, [PB[bk]], [B_junk, B_sm[s0]], accum=sm[:, s0:s0 + 1])
                rstd_from_ss(sm[:, s2:s2 + 1], sm[:, s0:s0 + 1], 256.0, [B_sm[s0], B_eps], [B_sm[s2]], sm[:, s1:s1 + 1], B_sm[s1])
                ts("dve", ckvn[i2][:, :], PS[bk][:, 0:256], sm[:, s2:s2 + 1], None, ALU.mult, None, [PB[bk], B_sm[s2]], [B_ckvn[i2]])
                act(junk[:, 256:320], PS[bk][:, 256:320], AF.Square, [PB[bk]], [B_junk, B_ssr[t]], accum=ssr[:, t:t + 1])
                tt("dve", xg[i2][:, :], PS[bk][:, 256:320], KNGr[:, :], ALU.mult, [PB[bk], B_KNGr], [B_xg[i2]])
                rope("pool", KRg[:, t, 0:32], KRg[:, t, 32:64], xg[i2], cosT[:, t, :], sinT[:, t, :], rt[i2],
                     [B_xg[i2], B_cos, B_sin], B_rt[i2], [B_KRg[t]])
                tb = 4 + i2
                pv = psbf(tb)
                for c in range(2):
                    tr(pv[:, c * 128:(c + 1) * 128], ckvn[i2][:, c * 128:(c + 1) * 128], ident_b[:, :], [B_ckvn[i2], B_identb], [PB[tb]])
                for c in range(2):
                    ts("dve", ckvnT[:, c, t * 128:(t + 1) * 128], pv[:, c * 128:(c + 1) * 128], colA[:, 52 + c:53 + c], None, ALU.mult, None,
                       [PB[tb], B_colA], [B_ckvnT[t]])
                if own:
                    qb = i2
                    for k in range(16):
                        mm(PS[qb][:, :], uT[:, k, t * 128:(t + 1) * 128], A0[:, k, :], k == 0, k == 15, [B_AR[0], B_uT[t]], [PB[qb]])
                    s3, s4, s5 = 3 + 8 * i2, 4 + 8 * i2, 5 + 8 * i2
                    act(junk[:, :], PS[qb][:, :], AF.Square, [PB[qb]], [B_junk, B_sm[s3]], accum=sm[:, s3:s3 + 1])
                    rstd_from_ss(sm[:, s5:s5 + 1], sm[:, s3:s3 + 1], 512.0, [B_sm[s3], B_eps], [B_sm[s5]], sm[:, s4:s4 + 1], B_sm[s4])
                    ts("dve", cqn[i2][:, :], PS[qb][:, :], sm[:, s5:s5 + 1], None, ALU.mult, None, [PB[qb], B_sm[s5]], [B_cqn[i2]])
                    tb2 = 6 + i2
                    pv2 = psbf(tb2)
                    for c in range(4):
                        tr(pv2[:, c * 128:(c + 1) * 128], cqn[i2][:, c * 128:(c + 1) * 128], ident_b[:, :], [B_cqn[i2], B_identb], [PB[tb2]])
                    for c in range(4):
                        o = cqnT[:, c, (t - 8) * 128:(t - 7) * 128]
                        if c % 2 == 0:
                            act(o, pv2[:, c * 128:(c + 1) * 128], AF.Identity, [PB[tb2], B_colA], [B_cqnT[t - 8]], scale=colA[:, 48 + c:49 + c])
                        else:
                            ts("dve", o, pv2[:, c * 128:(c + 1) * 128], colA[:, 48 + c:49 + c], None, ALU.mult, None, [PB[tb2], B_colA], [B_cqnT[t - 8]])

            if DBG_SUB == "A":
                P.finalize()
                return nc
            Wkv = ARENA[0][:, 0:4096].rearrange("p (c n) -> p c n", c=2)
            Wuq = ARENA[1][:, 0:6144].rearrange("p (c n) -> p c n", c=4)
            for c in range(2):
                P.dma("pool", (lambda c=c: (lambda hh: hh.dma_start(out=Wkv[:, c, :], in_=w_ukv_d[c * 128:(c + 1) * 128, :], max_dma_last_dim=4096)))(),
                      [], [B_AR[0]], sembuf=B_AR[0])
            for c in range(4):
                P.dma("pool", (lambda c=c: (lambda hh: hh.dma_start(out=Wuq[:, c, :], in_=w_uq_d[c * 128:(c + 1) * 128, :], max_dma_last_dim=3072)))(),
                      [], [B_AR[1]], sembuf=B_AR[1])
            KTn = sb(ph, "KTn", [128, NT * 128], BF16); B_KTn = [Buf("KTn%d" % t) for t in range(NT)]
            KTr = sb(ph, "KTr", [128, NT * 128], BF16); B_KTr = [Buf("KTr%d" % t) for t in range(NT)]
            VA = sb(ph, "VA", [128, NT, 132], BF16); B_VA = [Buf("VA%d" % t) for t in range(NT)]
            QTn = sb(ph, "QTn", [128, 1024], BF16); B_QTn = [Buf("QTn%d" % t) for t in range(NOWN)]
            QTr = sb(ph, "QTr", [128, 1024], BF16); B_QTr = [Buf("QTr%d" % t) for t in range(NOWN)]
            PT = [sb(ph, "PT%d" % i, [128, 512], BF16) for i in range(4)]; B_PT = [Buf("PT%d" % i) for i in range(4)]
            B_zpad = Buf("zpad")
            memset("pool", KTr[64:128, :], 0.0, [B_zpad])
            memset("pool", QTr[64:128, :], 0.0, [B_zpad])
            kn = [sb(ph, "kn%d" % i, [128, 128], BF16) for i in range(4)]; B_kn = [Buf("kn%d" % i) for i in range(4)]
            krn = [sb(ph, "krn%d" % i, [128, 64], BF16) for i in range(4)]; B_krn = [Buf("krn%d" % i) for i in range(4)]
            qn = [sb(ph, "qn%d" % i, [128, 128], BF16) for i in range(4)]; B_qn = [Buf("qn%d" % i) for i in range(4)]
            qr = [sb(ph, "qr%d" % i, [128, 64], BF16) for i in range(4)]; B_qr = [Buf("qr%d" % i) for i in range(4)]
            xq = [sb(ph, "xq%d" % i, [128, 64], F32) for i in range(4)]; B_xq = [Buf("xq%d" % i) for i in range(4)]
            rq = [sb(ph, "rq%d" % i, [128, 4, 32], F32) for i in range(4)]; B_rq = [Buf("rq%d" % i) for i in range(4)]
            KVB = [0, 1, 5, 6]
            TRB = [2, 7]
            tri_ = 0
            ssh = sb(ph, "ssh", [128, NOWN, 8], F32); B_ssh = [Buf("ssh%d" % t) for t in range(NOWN)]
            rden = sb(ph, "rden", [128, NOWN], F32); B_rden = [Buf("rden%d" % t) for t in range(NOWN)]
            for t in range(NT):
                if t >= 8:
                    memset("pool", VA[:, t, 128:129], 1.0, [B_VA[t]])
                else:
                    cp("pool", VA[:, t, 128:129], pm[:, 0:1], [B_pm], [B_VA[t]])
            pt_i = 0
            if DBG_SUB == "B0":
                P.finalize()
                return nc
            for h in range(8):
                for t in range(NT):
                    i2 = t % 4
                    bk = KVB[t % 4]
                    for c in range(2):
                        mm(PS[bk][:, 0:256], ckvnT[:, c, t * 128:(t + 1) * 128], Wkv[:, c, h * 256:(h + 1) * 256], c == 0, c == 1,
                           [B_AR[0], B_ckvnT[t]], [PB[bk]])
                    s0, s1, s2 = 16 + 4 * i2, 17 + 4 * i2, 18 + 4 * i2
                    act(junk[:, 0:128], PS[bk][:, 0:128], AF.Square, [PB[bk]], [B_junk, B_sm[s0]], accum=sm[:, s0:s0 + 1])
                    tt("dve", sm[:, s0:s0 + 1], sm[:, s0:s0 + 1], ssr[:, t:t + 1], ALU.add, [B_sm[s0], B_ssr[t]], [B_sm[s0]])
                    rstd_from_ss(sm[:, s2:s2 + 1], sm[:, s0:s0 + 1], 192.0, [B_sm[s0], B_eps], [B_sm[s2]], sm[:, s1:s1 + 1], B_sm[s1])
                    ts("dve", kn[i2][:, :], PS[bk][:, 0:128], sm[:, s2:s2 + 1], None, ALU.mult, None, [PB[bk], B_sm[s2]], [B_kn[i2]])
                    ts("pool", krn[i2][:, :], KRg[:, t, :], sm[:, s2:s2 + 1], None, ALU.mult, None, [B_KRg[t], B_sm[s2]], [B_krn[i2]])
                    if t >= 8:
                        cp("act", VA[:, t, 0:128], PS[bk][:, 128:256], [PB[bk]], [B_VA[t]])
                    else:
                        act(VA[:, t, 0:128], PS[bk][:, 128:256], AF.Identity, [PB[bk], B_pm], [B_VA[t]], scale=pm[:, 0:1])
                    tb = TRB[tri_ % 2]
                    tri_ += 1
                    pv = psbf(tb)
                    tr(pv[:, 0:128], kn[i2][:, :], ident_b[:, :], [B_kn[i2], B_identb], [PB[tb]])
                    tr(pv[0:64, 128:256], krn[i2][:, :], ident_b[:, :], [B_krn[i2], B_identb], [PB[tb]])
                    ts("dve", KTn[:, t * 128:(t + 1) * 128], pv[:, 0:128], colA[:, 56:57], None, ALU.mult, None, [PB[tb], B_colA], [B_KTn[t]])
                    cp("act", KTr[0:64, t * 128:(t + 1) * 128], pv[0:64, 128:256], [PB[tb]], [B_KTr[t]])
                if DBG_SUB == "B1k":
                    P.finalize()
                    return nc
                for t in range(NOWN):
                    i2 = t % 4
                    qb = 3 + t % 2
                    for c in range(4):
                        mm(PS[qb][:, 0:192], cqnT[:, c, t * 128:(t + 1) * 128], Wuq[:, c, h * 192:(h + 1) * 192], c == 0, c == 3,
                           [B_AR[1], B_cqnT[t]], [PB[qb]])
                    s0, s1, s2 = 32 + 4 * i2, 33 + 4 * i2, 34 + 4 * i2
                    act(junk[:, 0:192], PS[qb][:, 0:192], AF.Square, [PB[qb]], [B_junk, B_sm[s0]], accum=sm[:, s0:s0 + 1])
                    rstd_from_ss(sm[:, s2:s2 + 1], sm[:, s0:s0 + 1], 192.0, [B_sm[s0], B_eps], [B_sm[s2]], sm[:, s1:s1 + 1], B_sm[s1])
                    ts("dve", qn[i2][:, :], PS[qb][:, 0:128], sm[:, s2:s2 + 1], None, ALU.mult, None, [PB[qb], B_sm[s2]], [B_qn[i2]])
                    stt(xq[i2][:, :], PS[qb][:, 128:192], sm[:, s2:s2 + 1], QNGr[:, :], ALU.mult, ALU.mult, [PB[qb], B_sm[s2], B_QNGr], [B_xq[i2]])
                    rope("pool", qr[i2][:, 0:32], qr[i2][:, 32:64], xq[i2], cosT[:, 8 + t, :], sinT[:, 8 + t, :], rq[i2],
                         [B_xq[i2], B_cos, B_sin], B_rq[i2], [B_qr[i2]])
                    tb = TRB[tri_ % 2]
                    tri_ += 1
                    pv = psbf(tb)
                    tr(pv[:, 256:384], qn[i2][:, :], ident_b[:, :], [B_qn[i2], B_identb], [PB[tb]])
                    tr(pv[0:64, 384:512], qr[i2][:, :], ident_b[:, :], [B_qr[i2], B_identb], [PB[tb]])
                    ts("dve", QTn[:, t * 128:(t + 1) * 128], pv[:, 256:384], colA[:, 54:55], None, ALU.mult, None, [PB[tb], B_colA], [B_QTn[t]])
                    cp("act", QTr[0:64, t * 128:(t + 1) * 128], pv[0:64, 384:512], [PB[tb]], [B_QTr[t]])
                if DBG_SUB == "B1":
                    P.finalize()
                    return nc
                for bnk in (5, 6, 7):
                    P.op("dve", (lambda bnk=bnk: (lambda hh: hh.memset(PS[bnk][:, 0:387], 0.0)))(), [], [PB[bnk]])

                def Oap(i):
                    return PS[5 + i // 3][:, (i % 3) * 129:(i % 3) * 129 + 129]

                sbank = 0
                for j in range(NT):
                    qs = 0 if j < 8 else j - 8
                    c0 = qs * 128
                    while c0 < 1024:
                        n = min(512, 1024 - c0)
                        bk = 3 + sbank % 2
                        sbank += 1
                        qtiles = list(range(c0 // 128, (c0 + n) // 128))
                        mm(PS[bk][:, 0:n], KTn[:, j * 128:(j + 1) * 128], QTn[:, c0:c0 + n], True, False,
                           [B_KTn[j]] + [B_QTn[i] for i in qtiles], [PB[bk]])
                        mm(PS[bk][:, 0:n], KTr[:, j * 128:(j + 1) * 128], QTr[:, c0:c0 + n], False, True,
                           [B_KTr[j], B_zpad] + [B_QTr[i] for i in qtiles], [PB[bk]])
                        r = pt_i % 4
                        pt_i += 1
                        act(PT[r][:, 0:n], PS[bk][:, 0:n], AF.Exp, [PB[bk], B_eps], [B_PT[r]], scale=SCALE, bias=shift_t[:, 0:1])
                        if j >= 8 and c0 == qs * 128:
                            tt("pool", PT[r][:, 0:128], PT[r][:, 0:128], tri_b[:, :], ALU.mult, [B_PT[r], B_trib], [B_PT[r]])
                        for i in qtiles:
                            ob = 5 + i // 3
                            mm(Oap(i), PT[r][:, i * 128 - c0:i * 128 - c0 + 128], VA[:, j, 0:129], False, False,
                               [B_PT[r], B_VA[j]], [PB[ob]], skip=True)
                        c0 += n
                if DBG_SUB == "B2":
                    P.finalize()
                    return nc
                for i in range(NOWN):
                    ob = 5 + i // 3
                    O = Oap(i)
                    P.op("dve", (lambda i=i, O=O: (lambda hh: hh.reciprocal(out=rden[:, i:i + 1], in_=O[:, 128:129])))(), [PB[ob]], [B_rden[i]])
                    ts("dve", merged[:, i, 1024 + h * 128:1024 + (h + 1) * 128], O[:, 0:128], rden[:, i:i + 1], None, ALU.mult, None,
                       [PB[ob], B_rden[i]], [B_mg[i]])
                    act(junk[:, 0:128], O[:, 0:128], AF.Square, [PB[ob], B_rden[i]], [B_junk, B_ssh[i]], scale=rden[:, i:i + 1],
                        accum=ssh[:, i, h:h + 1])
            for i in range(NOWN):
                s0, s1, s2 = 48, 49, 50
                P.op("dve", (lambda i=i: (lambda hh: hh.tensor_reduce(out=sm[:, 48:49], in_=ssh[:, i, :], axis=AX.X, op=ALU.add)))(),
                     [B_ssh[i]], [B_sm[s0]])
                rstd_from_ss(sm[:, s2:s2 + 1], sm[:, s0:s0 + 1], 1024.0, [B_sm[s0], B_eps], [B_sm[s2]], sm[:, s1:s1 + 1], B_sm[s1])
                stt(merged[:, i, 1024:2048], merged[:, i, 1024:2048], sm[:, s2:s2 + 1], MOG[:, :], ALU.mult, ALU.mult,
                    [B_mg[i], B_sm[s2], B_MOG], [B_mg[i]])
            barrier()

        mx.close()

        if stage == 2:
            with ExitStack() as ph:
                dmp = sb(ph, "dmp", [128, NOWN, D], F32); B_dmp = Buf("dmp")
                for t in range(NOWN):
                    cp("dve", dmp[:, t, :], merged[:, t, :], [B_mg[t], B_dmp], [B_dmp])
                load("sp", dbg_d.rearrange("(t p) d -> p t d", p=128), dmp[:, :, :], [], B_dmp, reads=[B_dmp])
                P.op("sp", lambda h: h.nop(), [], [B_dmp])
                P.finalize()
            return nc

        hres = sb(st, "hres", [128, NOWN, D], F32); B_hres = [Buf("hres%d" % t) for t in range(NOWN)]
        GT = sb(st, "GT", [128, D], F32); B_GT = Buf("GT")
        Dg = [sb(st, "Dg%d" % i, [128, 128], F32) for i in range(2)]; B_Dg = [Buf("Dg0"), Buf("Dg1")]

        def row_broadcast(col_ap, B_col, bank0=0):
            for c in range(16):
                i2 = c % 2
                ts("dve", Dg[i2][:, :], ident_f[:, :], col_ap[:, c:c + 1], None, ALU.mult, None, [B_identf, B_col], [B_Dg[i2]])
                bk = bank0 + i2
                mm(PS[bk][:, 0:128], ones_f[:, :], Dg[i2][:, :], True, True, [B_ones, B_Dg[i2]], [PB[bk]])
                cp("act", GT[:, c * 128:(c + 1) * 128], PS[bk][:, 0:128], [PB[bk]], [B_GT])

        with ExitStack() as ph:
            mT = sb(ph, "mT", [128, 16, 1024], BF16); B_mT = [Buf("mT%d" % t) for t in range(NOWN)]
            xs = [sb(ph, "xs%d" % i, [128, 512], F32) for i in range(3)]; B_xs = [Buf("xs%d" % i) for i in range(3)]
            row_broadcast(modT[:, 32:48], B_mod2)
            for i in range(NOWN):
                for half in range(2):
                    bank = 4 + (2 * i + half) % 4
                    pv = psbf(bank)
                    for c8 in range(8):
                        c = half * 8 + c8
                        tr(pv[:, c8 * 128:(c8 + 1) * 128], merged[:, i, c * 128:(c + 1) * 128], ident_b[:, :], [B_mg[i], B_identb], [PB[bank]])
                    o = mT[:, half * 8:(half + 1) * 8, i * 128:(i + 1) * 128]
                    src = pv[:, :].rearrange("p (c x) -> p c x", c=8)
                    if half == 0:
                        cp("act", o, src, [PB[bank]], [B_mT[i]])
                    else:
                        cp("dve", o, src, [PB[bank]], [B_mT[i]])
            wo3 = w_out_d.rearrange("(k p) c -> p k c", p=128)
            xi = 0
            for n in range(4):
                Wo = ARENA[n % 2][:, :].rearrange("p (k c) -> p k c", k=16)
                bw = B_AR[n % 2]
                P.dma("pool", (lambda n=n, Wo=Wo: (lambda hh: hh.dma_start(out=Wo, in_=wo3[:, :, n * 512:(n + 1) * 512])))(), [], [bw], sembuf=bw)
                gtb = GT[:, n * 512:(n + 1) * 512].unsqueeze(1).to_broadcast([128, 16, 512])
                tt("pool", Wo, Wo, gtb, ALU.mult, [bw, B_GT], [bw])
                for i in range(NOWN):
                    x_ = xs[xi % 3]
                    Bx = B_xs[xi % 3]
                    xi += 1
                    load("sp", x_[:, :], xo_d[i * 128:(i + 1) * 128, n * 512:(n + 1) * 512], [Bx], Bx)
                    bk = (n * NOWN + i) % 4
                    for k in range(16):
                        mm(PS[bk][:, :], mT[:, k, i * 128:(i + 1) * 128], Wo[:, k, :], k == 0, k == 15, [bw, B_mT[i]], [PB[bk]])
                    tt("dve", hres[:, i, n * 512:(n + 1) * 512], PS[bk][:, :], x_[:, :], ALU.add, [PB[bk], Bx], [B_hres[i]])
            barrier()

        if stage == 3:
            load("sp", dbg_d.rearrange("(t p) d -> p t d", p=128), hres[:, :, :], [], B_hres[0], reads=B_hres)
            P.op("sp", lambda h: h.nop(), [], B_hres)
            P.finalize()
            return nc

        with ExitStack() as ph:
            u2T = sb(ph, "u2T", [128, 16, 1024], BF16); B_u2T = [Buf("u2T%d" % t) for t in range(NOWN)]
            wg = sb(ph, "wg", [128, NOWN, 64], F32); B_wg = [Buf("wg%d" % t) for t in range(NOWN)]
            row_broadcast(modT[:, 80:96], B_mod2, bank0=2)
            UN = [ARENA[0][:, :], ARENA[1][:, :],
                  merged[:, 0:4, :].rearrange("p a d -> p (a d)"), merged[:, 4:8, :].rearrange("p a d -> p (a d)")]
            B_UN = [B_AR[0], B_AR[1], Buf("un2"), Buf("un3")]
            gtb4 = GT[:, :].unsqueeze(1).to_broadcast([128, 4, D])
            n_tot = n_exp + 1

            def wsrc(e):
                if e < n_exp:
                    return w_g_d[e], w_u_d[e], w_d_d[e]
                return ws_g_d, ws_u_d, ws_d_d

            def issue_loads(e, which):
                g_d, u_d, d_d = wsrc(e)
                for i, src in ((0, g_d), (1, u_d)):
                    if i not in which:
                        continue
                    u = (3 * e + i) % 4
                    dst = UN[u].rearrange("p (k c) -> p k c", k=16)
                    P.dma("pool", (lambda dst=dst, src=src: (lambda hh: hh.dma_start(out=dst, in_=src.rearrange("(k p) c -> p k c", p=128))))(),
                          [], [B_UN[u]], sembuf=B_UN[u])
                if 2 not in which:
                    return
                u = (3 * e + 2) % 4
                dst = UN[u].rearrange("p (c n) -> p c n", c=4)
                for c in range(4):
                    P.dma("pool", (lambda dst=dst, c=c, d_d=d_d: (lambda hh: hh.dma_start(out=dst[:, c, :], in_=d_d[c * 128:(c + 1) * 128, :],
                                                                                           max_dma_last_dim=4096)))(),
                          [], [B_UN[u]], sembuf=B_UN[u])
                tt("pool", dst, dst, gtb4, ALU.mult, [B_UN[u], B_GT], [B_UN[u]])

            issue_loads(0, (0, 1, 2))
            with ExitStack() as ph2:
                norm_to_T(ph2, lambda t: (hres[:, t, :], B_hres[t]), NOWN, u2T, B_u2T, af_, modT[:, 48:64], B_af, B_mod2, "n2")
                Wr = sb(ph2, "Wr", [128, 16, 64], BF16); B_Wr = Buf("Wr")
                RB = sb(ph2, "RB", [128, 64], F32); B_RB = Buf("RB")
                P.dma("pool", lambda hh: hh.dma_start(out=Wr[:, :, :], in_=w_r_d.rearrange("(k p) e -> p k e", p=128)), [], [B_Wr], sembuf=B_Wr)
                load("sp", RB[:, :], rb_d.partition_broadcast(128), [B_RB], B_RB)
                sc = sb(ph2, "sc", [128, 64], F32); B_sc = Buf("sc")
                sel = sb(ph2, "sel", [128, 64], F32); B_sel = Buf("sel")
                selm = sb(ph2, "selm", [128, 64], F32); B_selm = Buf("selm")
                m8 = sb(ph2, "m8", [128, 8, 8], F32); B_m8 = Buf("m8")
                gs = sb(ph2, "gs", [128, 8], F32); B_gs = Buf("gs")
                gm8 = sb(ph2, "gm8", [128, 8], F32); B_gm8 = Buf("gm8")
                gmk = sb(ph2, "gmk", [128, 8], F32); B_gmk = Buf("gmk")
                pen = sb(ph2, "pen", [128, 8], F32); B_pen = Buf("pen")
                em8 = sb(ph2, "em8", [128, 8], F32); B_em8 = Buf("em8")
                emk = sb(ph2, "emk", [128, 64], F32); B_emk = Buf("emk")
                den = sb(ph2, "den", [128, 2], F32); B_den = Buf("den")
                for t in range(NOWN):
                    bk = t % 2
                    for k in range(16):
                        mm(PS[bk][:, 0:64], u2T[:, k, t * 128:(t + 1) * 128], Wr[:, k, :], k == 0, k == 15, [B_Wr, B_u2T[t]], [PB[bk]])
                    act(sc[:, :], PS[bk][:, 0:64], AF.Sigmoid, [PB[bk]], [B_sc])
                    tt("dve", sel[:, :], sc[:, :], RB[:, :], ALU.add, [B_sc, B_RB], [B_sel])
                    for g in range(8):
                        P.op("dve", (lambda g=g: (lambda hh: hh.max(out=m8[:, g, :], in_=sel[:, g * 8:(g + 1) * 8])))(), [B_sel], [B_m8])
                    tt("dve", gs[:, :], m8[:, :, 0], m8[:, :, 1], ALU.add, [B_m8], [B_gs])
                    P.op("dve", lambda hh: hh.max(out=gm8[:, :], in_=gs[:, :]), [B_gs], [B_gm8])
                    ts("dve", gmk[:, :], gs[:, :], gm8[:, 3:4], None, ALU.is_ge, None, [B_gs, B_gm8], [B_gmk])
                    ts("dve", pen[:, :], gmk[:, :], 4.0, -4.0, ALU.mult, ALU.add, [B_gmk], [B_pen])
                    sel3 = sel[:, :].rearrange("p (g e) -> p g e", g=8)
                    selm3 = selm[:, :].rearrange("p (g e) -> p g e", g=8)
                    tt("dve", selm3, sel3, gmk[:, :].unsqueeze(2).to_broadcast([128, 8, 8]), ALU.mult, [B_sel, B_gmk], [B_selm])
                    tt("dve", selm3, selm3, pen[:, :].unsqueeze(2).to_broadcast([128, 8, 8]), ALU.add, [B_selm, B_pen], [B_selm])
                    P.op("dve", lambda hh: hh.max(out=em8[:, :], in_=selm[:, :]), [B_selm], [B_em8])
                    ts("dve", emk[:, :], selm[:, :], em8[:, 7:8], None, ALU.is_ge, None, [B_selm, B_em8], [B_emk])
                    tt("dve", emk[:, :], emk[:, :], sc[:, :], ALU.mult, [B_emk, B_sc], [B_emk])
                    P.op("dve", lambda hh: hh.tensor_reduce(out=den[:, 0:1], in_=emk[:, :], axis=AX.X, op=ALU.add), [B_emk], [B_den])
                    ts("dve", den[:, 0:1], den[:, 0:1], 0.4, None, ALU.mult, None, [B_den], [B_den])
                    P.op("dve", lambda hh: hh.reciprocal(out=den[:, 1:2], in_=den[:, 0:1]), [B_den], [B_den])
                    ts("dve", wg[:, t, :], emk[:, :], den[:, 1:2], None, ALU.mult, None, [B_emk, B_den], [B_wg[t]])
                barrier()
                if DBG_SUB == "WG":
                    dbg2_d = nc.dram_tensor("dbg2", [128, 512], F32, kind="ExternalOutput").ap()
                    load("sp", dbg2_d[:, :], wg[:, :, :].rearrange("p t e -> p (t e)"), [], B_wg[0], reads=B_wg)
                    P.op("sp", lambda h: h.nop(), [], B_wg)

            hid = sb(ph, "hid", [128, 4, 1024], BF16); B_hid = [Buf("hidA"), Buf("hidB")]
            sgl = [sb(ph, "sgl%d" % i, [128, 512], F32) for i in range(2)]; B_sgl = [Buf("sgl0"), Buf("sgl1")]
            dbi = 0
            for e in range(n_tot):
                if e + 1 < n_tot:
                    issue_loads(e + 1, (0,))
                Wg_ = UN[(3 * e) % 4].rearrange("p (k c) -> p k c", k=16); Bg = B_UN[(3 * e) % 4]
                Wu_ = UN[(3 * e + 1) % 4].rearrange("p (k c) -> p k c", k=16); Bu = B_UN[(3 * e + 1) % 4]
                Wd_ = UN[(3 * e + 2) % 4].rearrange("p (c n) -> p c n", c=4); Bd = B_UN[(3 * e + 2) % 4]
                gi = 0
                for half in range(2):
                    tl = [B_u2T[half * 4 + i] for i in range(4)]
                    for hc in range(4):
                        gb = gi % 2
                        ub = 2 + gi % 2
                        gi += 1
                        for k in range(16):
                            mm(PS[gb][:, :], Wg_[:, k, hc * 128:(hc + 1) * 128], u2T[:, k, half * 512:(half + 1) * 512], k == 0, k == 15,
                               [Bg] + tl, [PB[gb]])
                        for k in range(16):
                            mm(PS[ub][:, :], Wu_[:, k, hc * 128:(hc + 1) * 128], u2T[:, k, half * 512:(half + 1) * 512], k == 0, k == 15,
                               [Bu] + tl, [PB[ub]])
                        act(sgl[gb][:, :], PS[gb][:, :], AF.Silu, [PB[gb]], [B_sgl[gb]])
                        tt("dve", hid[:, hc, half * 512:(half + 1) * 512], sgl[gb][:, :], PS[ub][:, :], ALU.mult, [B_sgl[gb], PB[ub]], [B_hid[half]])
                if e + 1 < n_tot:
                    issue_loads(e + 1, (1, 2))
                for t in range(NOWN):
                    for dg in range(4):
                        db = 4 + dbi % 4
                        dbi += 1
                        for hc in range(4):
                            mm(PS[db][:, :], hid[:, hc, t * 128:(t + 1) * 128], Wd_[:, hc, dg * 512:(dg + 1) * 512], hc == 0, hc == 3,
                               [B_hid[t // 4], Bd], [PB[db]])
                        hsl = hres[:, t, dg * 512:(dg + 1) * 512]
                        if e < n_exp:
                            stt(hsl, PS[db][:, :], wg[:, t, e:e + 1], hsl, ALU.mult, ALU.add, [PB[db], B_wg[t], B_hres[t]], [B_hres[t]])
                        else:
                            tt("dve", hsl, PS[db][:, :], hsl, ALU.add, [PB[db], B_hres[t]], [B_hres[t]])
            for t in range(NOWN):
                load("sp", out_d[t * 128:(t + 1) * 128, :], hres[:, t, :], [], B_hres[t], reads=[B_hres[t]])
            P.op("sp", lambda h: h.nop(), [], B_hres)
            P.finalize()
    return nc


def make_in_maps(inputs, stage=99):
    x = np.ascontiguousarray(inputs["x"], dtype=np.float32)
    c = np.asarray(inputs["c"], dtype=np.float32)
    pos = np.asarray(inputs["positions"], dtype=np.int32)
    maps = []
    zeros_x = np.zeros((1024, D), np.float32)
    for cid in range(8):
        b, s = cid // 2, cid % 2
        vecA = np.zeros((128, 128), np.float32)
        vecA[0:16] = c[b].reshape(16, 128)
        vecA[16:32] = np.asarray(inputs["norm_mix_g"], np.float32).reshape(16, 128)
        vecA[32:48] = np.asarray(inputs["norm_ffn_g"], np.float32).reshape(16, 128)
        vecA[48:52] = np.asarray(inputs["mla_q_a_g"], np.float32).reshape(4, 128)
        vecA[52:54] = np.asarray(inputs["mla_kv_a_g"], np.float32).reshape(2, 128)
        qn = np.asarray(inputs["mla_q_norm_g"], np.float32).reshape(192)
        kn = np.asarray(inputs["mla_k_norm_g"], np.float32).reshape(192)
        vecA[54, :] = qn[0:128]
        vecA[55, 0:64] = qn[128:192]
        vecA[56, :] = kn[0:128]
        vecA[57, 0:64] = kn[128:192]
        p16 = np.zeros((NT, 128), np.int32)
        if s == 1:
            p16[0:8] = pos[b, 0:1024].reshape(8, 128)
            p16[8:16] = pos[b, 1024:2048].reshape(8, 128)
        else:
            p16[8:16] = pos[b, 0:1024].reshape(8, 128)
        m = {
            "xp": x[b, 0:1024] if s == 1 else zeros_x,
            "xo": x[b, s * 1024:(s + 1) * 1024],
            "vecA": vecA,
            "vecB": np.asarray(inputs["b_ada"], np.float32).reshape(96, 128),
            "pos": np.ascontiguousarray(p16.T),
            "pmask": np.full((128, 1), float(s), np.float32),
            "w_ada": np.asarray(inputs["w_ada"], np.float32).reshape(D, 6 * D),
            "w_in": np.asarray(inputs["w_in"], np.float32).reshape(D, IN_COLS),
            "hg_lb_logits": np.asarray(inputs["hg_lb_logits"], np.float32),
            "hg_out_g": np.asarray(inputs["hg_out_g"], np.float32).reshape(128),
            "mla_w_uq": np.asarray(inputs["mla_w_uq"], np.float32).reshape(512, 1536),
            "mla_w_ukv": np.asarray(inputs["mla_w_ukv"], np.float32).reshape(256, 2048),
            "mla_q_norm_g": qn,
            "mla_k_norm_g": kn,
            "mla_out_g": np.asarray(inputs["mla_out_g"], np.float32).reshape(1024),
            "w_out": np.asarray(inputs["w_out"], np.float32).reshape(D, D),
        }
        if stage >= 4:
            m.update({
                "w_router": np.asarray(inputs["w_router"], np.float32).reshape(D, 64),
                "router_bias": np.asarray(inputs["router_bias"], np.float32).reshape(64),
                "w_gate": np.asarray(inputs["w_gate"], np.float32).reshape(N_EXP, D, 512),
                "w_up": np.asarray(inputs["w_up"], np.float32).reshape(N_EXP, D, 512),
                "w_down": np.asarray(inputs["w_down"], np.float32).reshape(N_EXP, 512, D),
                "ws_gate": np.asarray(inputs["ws_gate"], np.float32).reshape(D, 512),
                "ws_up": np.asarray(inputs["ws_up"], np.float32).reshape(D, 512),
                "ws_down": np.asarray(inputs["ws_down"], np.float32).reshape(512, D),
            })
        maps.append(m)
    return maps


def kernel(**inputs):
    nc = build_program(stage=99)
    in_maps = make_in_maps(inputs, stage=99)
    res = run_bass_kernel_spmd(nc, in_maps, core_ids=list(range(8)))
    out = np.empty((4, S, D), np.float32)
    for cid in range(8):
        b, s = cid // 2, cid % 2
        out[b, s * 1024:(s + 1) * 1024] = res.results[cid]["out"]
    return out
```

```python
import math
from contextlib import ExitStack

import numpy as np
import ml_dtypes

import concourse.bass as bass
import concourse.mybir as mybir
from concourse.bass_utils import run_bass_kernel_spmd

F32 = mybir.dt.float32
BF16 = mybir.dt.bfloat16
I32 = mybir.dt.int32
AF = mybir.ActivationFunctionType
ALU = mybir.AluOpType
AX = mybir.AxisListType

D = 2048
S = 2048
NT = 16
NOWN = 8
EPS = 1e-6
IN_COLS = 4928
N_EXP = 64
import os
DBG_SUB = os.environ.get("KSUB", "")
SCHED_WINDOW = int(os.environ.get("KWIN", "16"))
SM_SHIFT = -14.0


class Buf:
    __slots__ = ("name", "w", "r", "excl")

    def __init__(self, name, excl=False):
        self.name = name
        self.w = None
        self.r = []
        self.excl = excl


class Ins:
    __slots__ = ("eng", "fn", "deps", "signal", "sigval", "is_dma", "dsem", "dval", "alldeps", "idx", "cost", "fence", "fin")

    def __init__(self, eng, fn):
        self.eng = eng
        self.fn = fn
        self.alldeps = []
        self.idx = 0
        self.cost = 0.3
        self.fence = False
        self.fin = None
        self.deps = []
        self.signal = False
        self.sigval = None
        self.is_dma = False
        self.dsem = None
        self.dval = None


class Prog:
    ENGS = ["pe", "act", "dve", "pool", "sp"]

    def __init__(self, nc, stack):
        self.nc = nc
        self.stack = stack
        self.streams = {e: [] for e in self.ENGS}
        self.dma_sems = {}
        self.nsem = 0
        self.last = {e: None for e in self.ENGS}
        self.dma_last = {}
        self.all = []
        self.segs = [0]

    def new_sem(self, name):
        self.nsem += 1
        return self.stack.enter_context(self.nc.semaphore(name))

    def _add(self, eng, fn, reads, writes, extra=(), cost=0.3):
        ins = Ins(eng, fn)
        ins.cost = cost
        deps = list(extra)
        for b in reads:
            if b.w is not None:
                deps.append(b.w)
            if b.excl:
                deps.extend(r for r in b.r if r.eng != eng)
        for b in writes:
            if b.w is not None:
                deps.append(b.w)
            deps.extend(b.r)
        seen = set()
        dd = []
        for d in deps:
            if id(d) in seen:
                continue
            seen.add(id(d))
            dd.append(d)
        ins.alldeps = dd
        for b in reads:
            b.r.append(ins)
        for b in writes:
            b.w = ins
            b.r = []
        ins.idx = len(self.all)
        self.all.append(ins)
        return ins

    def op(self, eng, fn, reads=(), writes=(), cost=0.3):
        ins = self._add(eng, fn, list(reads), list(writes), cost=cost)
        self.last[eng] = ins
        return ins

    def dma(self, eng, fn, reads=(), writes=(), sembuf=None, cost=8.0):
        ins = self._add(eng, fn, list(reads), list(writes), cost=cost)
        ins.is_dma = True
        if sembuf not in self.dma_sems:
            self.dma_sems[sembuf] = [self.new_sem("d%d" % self.nsem), 0]
        ent = self.dma_sems[sembuf]
        ent[1] += 16
        ins.dsem = ent[0]
        ins.dval = ent[1]
        self.dma_last[sembuf] = ins
        return ins

    def barrier(self, tiny):
        self.segs.append(len(self.all))
        out = {}
        for e in self.ENGS:
            ins = self._add(e, tiny[e], [], [], cost=0.1)
            ins.fence = True
            out[e] = ins
        self.segs.append(len(self.all))
        return out

    def schedule(self, window=SCHED_WINDOW, lat=0.6):
        bounds = self.segs + [len(self.all)]
        streams = {e: [] for e in self.ENGS}
        prev_last = {}
        outstanding_dma = {}
        tbase = 0.0
        for si in range(len(bounds) - 1):
            seg = self.all[bounds[si]:bounds[si + 1]]
            if not seg:
                continue
            if seg[0].fence:
                deps = list(prev_last.values()) + list(outstanding_dma.values())
                for ins in seg:
                    ins.alldeps = [d for d in deps]
                    ins.fin = tbase
                    streams[ins.eng].append(ins)
                outstanding_dma = {}
                continue
            pend = {e: [i for i in seg if i.eng == e] for e in self.ENGS}
            pos = {e: 0 for e in self.ENGS}
            done = {e: [False] * len(pend[e]) for e in self.ENGS}
            free = {e: tbase for e in self.ENGS}
            left = len(seg)
            while left:
                best = None
                for e in self.ENGS:
                    lst = pend[e]
                    p = pos[e]
                    n = len(lst)
                    cnt = 0
                    j = p
                    seen_dma = False
                    while j < n and cnt < window:
                        if not done[e][j]:
                            ins = lst[j]
                            cnt += 1
                            if ins.is_dma:
                                if seen_dma:
                                    j += 1
                                    continue
                                seen_dma = True
                            ok = True
                            rt = free[e]
                            for d in ins.alldeps:
                                if d.fin is None:
                                    ok = False
                                    break
                                t_ = d.fin + (lat if d.eng != e or d.is_dma else 0.0)
                                if t_ > rt:
                                    rt = t_
                            if ok and (best is None or rt < best[0] - 1e-9 or (abs(rt - best[0]) <= 1e-9 and ins.idx < best[3].idx)):
                                best = (rt, e, j, ins)
                        j += 1
                rt, e, j, ins = best
                done[e][j] = True
                while pos[e] < len(pend[e]) and done[e][pos[e]]:
                    pos[e] += 1
                if ins.is_dma:
                    free[e] = rt + 0.15
                    ins.fin = rt + ins.cost
                else:
                    free[e] = rt + ins.cost
                    ins.fin = free[e]
                streams[e].append(ins)
                left -= 1
            for e in self.ENGS:
                if pend[e]:
                    ld = [i for i in streams[e] if not i.is_dma]
                    if ld:
                        prev_last[e] = ld[-1]
            for i in seg:
                if i.is_dma:
                    outstanding_dma[id(i.dsem)] = i if (id(i.dsem) not in outstanding_dma or outstanding_dma[id(i.dsem)].dval < i.dval) else outstanding_dma[id(i.dsem)]
            tbase = max(i.fin for i in seg)
        self.streams = streams
        self.sim_time = tbase

    def finalize(self):
        nc = self.nc
        LIMIT = 1 << 30
        self.schedule()
        for ins in self.all:
            dd = []
            for d in ins.alldeps:
                if d.eng == "pe" and ins.eng == "pe" and not d.is_dma:
                    continue
                dd.append(d)
                d.signal = True
            ins.deps = dd
        for e in self.ENGS:
            cnt = 0
            sem = None
            for ins in self.streams[e]:
                if ins.is_dma:
                    continue
                if ins.signal:
                    if sem is None or cnt >= LIMIT:
                        sem = self.new_sem("s%s%d" % (e, self.nsem))
                        cnt = 0
                    cnt += 1
                    ins.sigval = (sem, cnt)
        with nc.Block() as block:
            def make(e):
                def body(h):
                    waited = {}
                    for ins in self.streams[e]:
                        for d in ins.deps:
                            sem, val = (d.dsem, d.dval) if d.is_dma else d.sigval
                            if waited.get(id(sem), 0) >= val:
                                continue
                            h.wait_ge(sem, val)
                            waited[id(sem)] = val
                        r = ins.fn(h)
                        if ins.is_dma:
                            r.then_inc(ins.dsem, 16)
                        elif ins.signal:
                            r.then_inc(ins.sigval[0], 1)
                return body
            block.tensor(make("pe"))
            block.scalar(make("act"))
            block.vector(make("dve"))
            block.gpsimd(make("pool"))
            block.sync(make("sp"))


def build_program(stage=99, n_exp=N_EXP):
    nc = bass.Bass("TRN2", target_bir_lowering=False)

    def din(name, shape, dt=F32):
        return nc.dram_tensor(name, list(shape), dt, kind="ExternalInput").ap()

    xp_d = din("xp", [1024, D])
    xo_d = din("xo", [1024, D])
    vecA_d = din("vecA", [128, 128])
    vecB_d = din("vecB", [96, 128])
    pos_d = din("pos", [128, NT], F32 if False else I32)
    pm_d = din("pmask", [128, 1])
    w_ada_d = din("w_ada", [D, 6 * D])
    w_in_d = din("w_in", [D, IN_COLS])
    lbl_d = din("hg_lb_logits", [2, 1024])
    outg_d = din("hg_out_g", [128])
    w_uq_d = din("mla_w_uq", [512, 1536])
    w_ukv_d = din("mla_w_ukv", [256, 2048])
    qng_d = din("mla_q_norm_g", [192])
    kng_d = din("mla_k_norm_g", [192])
    mog_d = din("mla_out_g", [1024])
    w_out_d = din("w_out", [D, D])
    if stage >= 4:
        w_r_d = din("w_router", [D, 64])
        rb_d = din("router_bias", [64])
        w_g_d = din("w_gate", [N_EXP, D, 512])
        w_u_d = din("w_up", [N_EXP, D, 512])
        w_d_d = din("w_down", [N_EXP, 512, D])
        ws_g_d = din("ws_gate", [D, 512])
        ws_u_d = din("ws_up", [D, 512])
        ws_d_d = din("ws_down", [512, D])
    out_d = nc.dram_tensor("out", [1024, D], F32, kind="ExternalOutput").ap()
    dbg_d = None
    if stage < 4:
        dbg_d = nc.dram_tensor("dbg", [1024, D], F32, kind="ExternalOutput").ap()

    with ExitStack() as st:
        P = Prog(nc, st)

        def sb(stack, name, shape, dt):
            return stack.enter_context(nc.sbuf_tensor(name, list(shape), dt))

        PS = [st.enter_context(nc.psum_tensor("ps%d" % i, [128, 512], F32)) for i in range(8)]
        PB = [Buf("pb%d" % i, excl=True) for i in range(8)]

        def psbf(i):
            return PS[i][:, :].bitcast(BF16)

        ones_f = sb(st, "ones_f", [128, 128], F32); B_ones = Buf("ones")
        ident_f = sb(st, "ident_f", [128, 128], F32); B_identf = Buf("identf")
        ident_b = sb(st, "ident_b", [128, 128], BF16); B_identb = Buf("identb")
        tri_f = sb(st, "tri_f", [128, 128], F32); B_trif = Buf("trif")
        trs_f = sb(st, "trs_f", [128, 128], F32); B_trsf = Buf("trsf")
        tri_b = sb(st, "tri_b", [128, 128], BF16); B_trib = Buf("trib")
        tiny_t = sb(st, "tiny_t", [128, 8], F32)
        colA = sb(st, "colA", [128, 128], F32); B_colA = Buf("colA")
        colB = sb(st, "colB", [128, 96], F32); B_colB = Buf("colB")
        modT = sb(st, "modT", [128, 96], F32); B_mod1 = Buf("mod1"); B_mod2 = Buf("mod2")
        am = sb(st, "am", [128, 16], F32); B_am = Buf("am")
        af_ = sb(st, "af", [128, 16], F32); B_af = Buf("af")
        condT = sb(st, "condT", [128, 16], BF16); B_cond = Buf("cond")
        pm = sb(st, "pm", [128, 1], F32); B_pm = Buf("pm")
        posf = sb(st, "posf", [128, NT], F32); B_posf = Buf("posf")
        cosT = sb(st, "cosT", [128, NT, 32], F32); B_cos = Buf("cos")
        sinT = sb(st, "sinT", [128, NT, 32], F32); B_sin = Buf("sin")

        def tiny_fns():
            return {
                "pe": lambda h: h.matmul(PS[7][0:8, 0:8], lhsT=ident_b[:, 0:8], rhs=ident_b[:, 0:8], start=True, stop=True),
                "act": lambda h: h.copy(out=tiny_t[:, 0:1], in_=tiny_t[:, 1:2]),
                "dve": lambda h: h.tensor_copy(out=tiny_t[:, 2:3], in_=tiny_t[:, 3:4]),
                "pool": lambda h: h.tensor_copy(out=tiny_t[:, 4:5], in_=tiny_t[:, 5:6]),
                "sp": lambda h: h.nop(),
            }

        def barrier():
            fz = P.barrier(tiny_fns())
            for b in PB:
                b.w = None
                b.r = []
            PB[7].w = fz["pe"]

        def fsz(ap):
            n = 1
            for d in ap.shape[1:]:
                n *= int(d)
            return n

        def ecost(eng, ap):
            n = fsz(ap)
            if eng == "pool":
                return 0.25 + n / 500.0
            if eng == "act":
                return 0.2 + n / 1100.0
            return 0.12 + n / 950.0

        def mm(out, lhsT, rhs, start, stop, reads, writes, skip=False):
            return P.op("pe", lambda h: h.matmul(out, lhsT=lhsT, rhs=rhs, start=start, stop=stop,
                                                 skip_group_check=skip), reads, writes, cost=0.06 + fsz(rhs) / 2300.0)

        def tr(out, in_, ident, reads, writes):
            return P.op("pe", lambda h: h.transpose(out, in_, ident), reads, writes, cost=0.12)

        def act(out, in_, func, reads, writes, scale=None, bias=None, accum=None):
            kw = {}
            if scale is not None:
                kw["scale"] = scale
            if bias is not None:
                kw["bias"] = bias
            if accum is not None:
                kw["accum_out"] = accum
            return P.op("act", lambda h: h.activation(out=out, in_=in_, func=func, **kw), reads, writes, cost=ecost("act", out))

        def tt(eng, out, in0, in1, op, reads, writes):
            return P.op(eng, lambda h: h.tensor_tensor(out=out, in0=in0, in1=in1, op=op), reads, writes, cost=ecost(eng, out))

        def ts(eng, out, in0, s1, s2, op0, op1, reads, writes, accum=None):
            c = ecost(eng, out)
            if op1 is None:
                return P.op(eng, lambda h: h.tensor_scalar(out=out, in0=in0, scalar1=s1, scalar2=None, op0=op0), reads, writes, cost=c)
            if accum is not None:
                return P.op(eng, lambda h: h.tensor_scalar(out=out, in0=in0, scalar1=s1, scalar2=s2, op0=op0, op1=op1, accum_out=accum), reads, writes, cost=c)
            return P.op(eng, lambda h: h.tensor_scalar(out=out, in0=in0, scalar1=s1, scalar2=s2, op0=op0, op1=op1), reads, writes, cost=c)

        def stt(out, in0, scalar, in1, op0, op1, reads, writes):
            return P.op("dve", lambda h: h.scalar_tensor_tensor(out=out, in0=in0, scalar=scalar, in1=in1, op0=op0, op1=op1), reads, writes,
                        cost=ecost("dve", out))

        def cp(eng, out, in_, reads, writes):
            if eng == "act":
                return P.op("act", lambda h: h.copy(out=out, in_=in_), reads, writes, cost=ecost("act", out))
            return P.op(eng, lambda h: h.tensor_copy(out=out, in_=in_), reads, writes, cost=ecost(eng, out))

        def memset(eng, ap, val, writes):
            return P.op(eng, lambda h: h.memset(ap, val), [], writes, cost=ecost(eng, ap))

        def load(eng, out, in_, writes, sembuf, reads=()):
            return P.dma(eng, lambda h: h.dma_start(out=out, in_=in_), reads, writes, sembuf=sembuf, cost=2.0 + fsz(out) * 128 * 4 / 300e3)

        def rstd_from_ss(out, ss, n, reads, writes, tmp, Btmp):
            act(tmp, ss, AF.Ln, reads, [Btmp], scale=1.0 / n, bias=eps_t[:, 0:1])
            act(out, tmp, AF.Exp, [Btmp], writes, scale=-0.5)

        eps_t = sb(st, "eps_t", [128, 1], F32); B_eps = Buf("eps")
        one_t = sb(st, "one_t", [128, 1], F32)
        shift_t = sb(st, "shift_t", [128, 1], F32)
        memset("pool", tiny_t[:, :], 0.0, [])
        memset("pool", eps_t[:, :], EPS, [B_eps])
        memset("pool", one_t[:, :], 1.0, [B_eps])
        memset("pool", shift_t[:, :], SM_SHIFT, [B_eps])
        memset("pool", ones_f[:, :], 1.0, [B_ones])
        P.op("pool", lambda h: h.affine_select(out=ident_f[:, :], in_=ones_f[:, :], pattern=[[1, 128]], compare_op=ALU.is_equal,
                                               fill=0.0, base=0, channel_multiplier=-1), [B_ones], [B_identf])
        P.op("pool", lambda h: h.affine_select(out=tri_f[:, :], in_=ones_f[:, :], pattern=[[1, 128]], compare_op=ALU.is_ge,
                                               fill=0.0, base=0, channel_multiplier=-1), [B_ones], [B_trif])
        P.op("pool", lambda h: h.affine_select(out=trs_f[:, :], in_=ones_f[:, :], pattern=[[-1, 128]], compare_op=ALU.is_gt,
                                               fill=0.0, base=0, channel_multiplier=1), [B_ones], [B_trsf])
        cp("pool", ident_b[:, :], ident_f[:, :], [B_identf], [B_identb])
        cp("pool", tri_b[:, :], tri_f[:, :], [B_trif], [B_trib])
        load("sp", pm[:, :], pm_d[:, :], [B_pm], B_pm)

        merged = sb(st, "merged", [128, NOWN, D], BF16)
        B_mg = [Buf("mg%d" % t) for t in range(NOWN)]
        ARENA = [sb(st, "arena%d" % i, [128, 8192], BF16) for i in range(2)]
        B_AR = [Buf("ar0"), Buf("ar1")]
        mx = ExitStack()
        st.enter_context(mx)
        uT = sb(mx, "uT", [128, 16, NT * 128], BF16)
        B_uT = [Buf("uT%d" % t) for t in range(NT)]

        ph1 = ExitStack()
        st.enter_context(ph1)
        if True:
            ph = ph1
            vA = sb(ph, "vA", [128, 128], F32); B_vA = Buf("vA")
            vB = sb(ph, "vB", [96, 128], F32); B_vB = Buf("vB")
            posi = sb(ph, "posi", [128, NT], I32); B_posi = Buf("posi")
            load("sp", vA[:, :], vecA_d[:, :], [B_vA], B_vA)
            load("sp", vB[:, :], vecB_d[:, :], [B_vB], B_vB)
            load("sp", posi[:, :], pos_d[:, :], [B_posi], B_posi)
            tr(PS[0][:, 0:128], vA[:, :], ident_f[:, :], [B_vA, B_identf], [PB[0]])
            cp("dve", colA[:, :], PS[0][:, 0:128], [PB[0]], [B_colA])
            tr(PS[1][:, 0:96], vB[:, :], ident_f[0:96, 0:96], [B_vB, B_identf], [PB[1]])
            cp("dve", colB[:, :], PS[1][:, 0:96], [PB[1]], [B_colB])
            cp("dve", posf[:, :], posi[:, :], [B_posi], [B_posf])
            ctmp = sb(ph, "ctmp", [128, 16], F32); B_ctmp = Buf("ctmp")
            act(ctmp[:, :], colA[:, 0:16], AF.Exp, [B_colA], [B_ctmp], scale=-1.0)
            ts("dve", ctmp[:, :], ctmp[:, :], 1.0, None, ALU.add, None, [B_ctmp], [B_ctmp])
            P.op("dve", lambda h: h.reciprocal(out=ctmp[:, :], in_=ctmp[:, :]), [B_ctmp], [B_ctmp])
            tt("dve", condT[:, :], ctmp[:, :], colA[:, 0:16], ALU.mult, [B_ctmp, B_colA], [B_cond])

            jf = sb(ph, "jf", [128, 32], F32); B_jf = Buf("jf")
            ji = sb(ph, "ji", [128, 32], I32); B_ji = Buf("ji")
            P.op("pool", lambda h: h.iota(ji[:, :], pattern=[[1, 32]], base=0, channel_multiplier=0), [], [B_ji])
            cp("dve", jf[:, :], ji[:, :], [B_ji], [B_jf])
            invf = sb(ph, "invf", [128, 32], F32); B_invf = Buf("invf")
            l2p = sb(ph, "l2p", [128, 1], F32)
            memset("pool", l2p[:, :], -math.log(2.0 * math.pi), [B_invf])
            act(invf[:, :], jf[:, :], AF.Exp, [B_jf, B_invf], [B_invf], scale=-math.log(10000.0) / 32.0, bias=l2p[:, 0:1])
            turn = sb(ph, "turn", [128, NT, 32], F32); B_turn = Buf("turn")
            turi = sb(ph, "turi", [128, NT, 32], I32); B_turi = Buf("turi")
            turf = sb(ph, "turf", [128, NT, 32], F32); B_turf = Buf("turf")
            msk = sb(ph, "msk", [128, NT, 32], F32); B_msk = Buf("msk")
            for t in range(NT):
                ts("dve", turn[:, t, :], invf[:, :], posf[:, t:t + 1], None, ALU.mult, None, [B_invf, B_posf], [B_turn])
            for which, dst, B_dst in ((0, sinT, B_sin), (1, cosT, B_cos)):
                if which == 1:
                    ts("dve", turn[:, :, :], turn[:, :, :], 0.25, None, ALU.add, None, [B_turn], [B_turn])
                cp("dve", turi[:, :, :], turn[:, :, :], [B_turn], [B_turi])
                cp("dve", turf[:, :, :], turi[:, :, :], [B_turi], [B_turf])
                tt("dve", turf[:, :, :], turn[:, :, :], turf[:, :, :], ALU.subtract, [B_turn, B_turf], [B_turf])
                ts("dve", msk[:, :, :], turf[:, :, :], 0.5, None, ALU.is_gt, None, [B_turf], [B_msk])
                tt("dve", turf[:, :, :], turf[:, :, :], msk[:, :, :], ALU.subtract, [B_turf, B_msk], [B_turf])
                ts("dve", msk[:, :, :], turf[:, :, :], -0.5, None, ALU.is_lt, None, [B_turf], [B_msk])
                tt("dve", turf[:, :, :], turf[:, :, :], msk[:, :, :], ALU.add, [B_turf, B_msk], [B_turf])
                act(dst[:, :, :], turf[:, :, :], AF.Sin, [B_turf], [B_dst], scale=2.0 * math.pi)

            WA = [ARENA[i][:, :].rearrange("p (k c) -> p k c", k=16) for i in range(2)]
            B_WA = B_AR
            wa_v = w_ada_d.rearrange("(k p) c -> p k c", p=128)

            def ada_group(g):
                wb = WA[g % 2]
                bw = B_WA[g % 2]
                P.dma("pool", lambda hh: hh.dma_start(out=wb, in_=wa_v[:, :, g * 512:(g + 1) * 512]), [], [bw], sembuf=bw, cost=16.0)
                bank = 2 if g < 8 else 3
                for jj in range(4):
                    j = g * 4 + jj
                    for k in range(16):
                        mm(PS[bank][:, j:j + 1], wb[:, k, jj * 128:(jj + 1) * 128], condT[:, k:k + 1], k == 0, k == 15,
                           [bw, B_cond], [PB[bank]])

            for g in range(8):
                ada_group(g)
            tt("dve", modT[:, 0:32], PS[2][:, 0:32], colB[:, 0:32], ALU.add, [PB[2], B_colB], [B_mod1])
            ts("dve", am[:, :], modT[:, 16:32], 1.0, None, ALU.add, None, [B_mod1], [B_am])
            tt("dve", am[:, :], am[:, :], colA[:, 16:32], ALU.mult, [B_am, B_colA], [B_am])

            def ada_finish():
                tt("dve", modT[:, 32:96], PS[3][:, 32:96], colB[:, 32:96], ALU.add, [PB[3], B_colB], [B_mod2])
                ts("dve", af_[:, :], modT[:, 64:80], 1.0, None, ALU.add, None, [B_mod2], [B_af])
                tt("dve", af_[:, :], af_[:, :], colA[:, 32:48], ALU.mult, [B_af, B_colA], [B_af])

        def norm_to_T(ph, src_fn, ntiles, dstT, B_dst, a_col, s_col, B_a, B_s, tag, src_reads=None, after_tile=None):
            yb = [sb(ph, tag + "yb%d" % i, [128, D], BF16) for i in range(2)]
            B_yb = [Buf("yb0"), Buf("yb1")]
            ssq = sb(ph, tag + "ssq", [128, ntiles], F32)
            rsd = sb(ph, tag + "rsd", [128, ntiles], F32)
            lnt = sb(ph, tag + "lnt", [128, ntiles], F32)
            for t in range(ntiles):
                xa, B_x = src_fn(t)
                y = yb[t % 2]
                By = B_yb[t % 2]
                B_ss = Buf("ss")
                B_rs = Buf("rs")
                B_ln = Buf("ln")
                act(y[:, :], xa, AF.Square, [B_x], [By, B_ss], accum=ssq[:, t:t + 1])
                rstd_from_ss(rsd[:, t:t + 1], ssq[:, t:t + 1], float(D), [B_ss, B_eps], [B_rs], lnt[:, t:t + 1], B_ln)
                ts("dve", y[:, :], xa, rsd[:, t:t + 1], None, ALU.mult, None, [B_x, B_rs, By], [By])
                for half in range(2):
                    bank = 4 + (2 * t + half) % 4
                    pv = psbf(bank)
                    for c8 in range(8):
                        c = half * 8 + c8
                        tr(pv[:, c8 * 128:(c8 + 1) * 128], y[:, c * 128:(c + 1) * 128], ident_b[:, :], [By, B_identb], [PB[bank]])
                    if half == 0:
                        B_half = Buf("evA")
                    for c8 in range(8):
                        c = half * 8 + c8
                        o = dstT[:, c, t * 128:(t + 1) * 128]
                        i_ = pv[:, c8 * 128:(c8 + 1) * 128]
                        if half == 0:
                            act(o, i_, AF.Identity, [PB[bank], B_a, B_s], [B_half], scale=a_col[:, c:c + 1], bias=s_col[:, c:c + 1])
                        elif c8 < 7:
                            ts("dve", o, i_, a_col[:, c:c + 1], s_col[:, c:c + 1], ALU.mult, ALU.add, [PB[bank], B_a, B_s], [B_dst[t]])
                        else:
                            ts("dve", o, i_, a_col[:, c:c + 1], s_col[:, c:c + 1], ALU.mult, ALU.add, [PB[bank], B_a, B_s, B_half], [B_dst[t]])
                if after_tile is not None:
                    after_tile(t)

        with ExitStack() as ph:
            xt = [sb(ph, "xt%d" % i, [128, D], F32) for i in range(2)]
            B_xt = [Buf("xt0"), Buf("xt1")]

            def src_fn(t):
                src = xp_d if t < 8 else xo_d
                r0 = (t % 8) * 128
                load("sp", xt[t % 2][:, :], src[r0:r0 + 128, :], [B_xt[t % 2]], B_xt[t % 2])
                return xt[t % 2][:, :], B_xt[t % 2]

            norm_to_T(ph, src_fn, NT, uT, B_uT, am, modT, B_am, B_mod1, "n1", after_tile=lambda t: ada_group(8 + t))
            ada_finish()
            barrier()
        ph1.close()

        with ExitStack() as ph:
            omlb = sb(ph, "omlb", [128, 1024], F32); B_omlb = Buf("omlb")
            l1t = sb(ph, "l1t", [128, 1024], F32); B_l1t = Buf("l1t")
            outg = sb(ph, "outg", [128, 128], F32); B_outg = Buf("outg")
            load("sp", omlb[:, :], lbl_d[0, :].partition_broadcast(128), [B_omlb], B_omlb)
            load("sp", l1t[:, :], lbl_d[1, :].partition_broadcast(128), [B_l1t], B_l1t)
            load("sp", outg[:, :], outg_d.partition_broadcast(128), [B_outg], B_outg)
            tt("dve", omlb[:, :], omlb[:, :], l1t[:, :], ALU.subtract, [B_omlb, B_l1t], [B_omlb])
            act(omlb[:, :], omlb[:, :], AF.Exp, [B_omlb], [B_omlb])
            ts("dve", omlb[:, :], omlb[:, :], 1.0, None, ALU.add, None, [B_omlb], [B_omlb])
            P.op("dve", lambda h: h.reciprocal(out=omlb[:, :], in_=omlb[:, :]), [B_omlb], [B_omlb])

            hqT = sb(ph, "hqT", [128, 1024], F32); B_hqT = [Buf("hqT0"), Buf("hqT1")]
            kt = sb(ph, "kt", [128, NT, 128], F32); B_kt = [Buf("kt%d" % t) for t in range(NT)]
            lf = sb(ph, "lf", [128, NT, 128], F32); B_lf = Buf("lf")
            vv = sb(ph, "vv", [128, NT, 128], BF16); B_vv = [Buf("vv%d" % t) for t in range(NT)]
            sg = sb(ph, "sg", [128, NOWN, 128], F32); B_sg = [Buf("sg%d" % t) for t in range(NOWN)]
            sgm = [sb(ph, "sgm%d" % i, [128, 128], F32) for i in range(2)]; B_sgm = [Buf("sgm0"), Buf("sgm1")]
            S_f = sb(ph, "S_f", [128, 128], F32); B_S = Buf("S")
            S_b = sb(ph, "S_b", [128, 128], BF16); B_Sb = Buf("Sb")
            R = 3
            ec = [sb(ph, "ec%d" % i, [128, 128], F32) for i in range(R)]; B_ec = [Buf("ec%d" % i) for i in range(R)]
            eb = [sb(ph, "eb%d" % i, [128, 128], F32) for i in range(R)]; B_eb = [Buf("eb%d" % i) for i in range(R)]
            enb = [sb(ph, "enb%d" % i, [128, 128], F32) for i in range(R)]; B_enb = [Buf("enb%d" % i) for i in range(R)]
            dec = [sb(ph, "dec%d" % i, [128, 1], F32) for i in range(R)]; B_dec = [Buf("dec%d" % i) for i in range(R)]
            Kh = [sb(ph, "Kh%d" % i, [128, 128], BF16) for i in range(R)]; B_Kh = [Buf("Kh%d" % i) for i in range(R)]
            Qt = [sb(ph, "Qt%d" % i, [128, 128], BF16) for i in range(R)]; B_Qt = [Buf("Qt%d" % i) for i in range(R)]
            Kt = [sb(ph, "Kt%d" % i, [128, 128], BF16) for i in range(R)]; B_Kt = [Buf("Kt%d" % i) for i in range(R)]
            ATm = [sb(ph, "ATm%d" % i, [128, 128], BF16) for i in range(R)]; B_ATm = [Buf("ATm%d" % i) for i in range(R)]
            osq = sb(ph, "osq", [128, 128], F32); B_osq = Buf("osq")
            oss = sb(ph, "oss", [128, 4], F32)
            B_o1 = Buf("o1"); B_o2 = Buf("o2"); B_o3 = Buf("o3")

            w4 = w_in_d[:, 0:4096].rearrange("(k p) (j x) -> p k j x", p=128, x=1024)

            w3 = w_in_d.rearrange("(k p) c -> p k c", p=128)

            def load_head_w(h):
                dst = ARENA[h % 2][:, :].rearrange("p (k c) -> p k c", k=16)
                for j in range(4):
                    c0 = j * 1024 + h * 128
                    P.dma("pool", (lambda j=j, c0=c0: (lambda hh: hh.dma_start(out=dst[:, :, j * 128:(j + 1) * 128],
                                                                              in_=w3[:, :, c0:c0 + 128])))(),
                          [], [B_AR[h % 2]], sembuf=B_AR[h % 2])

            load_head_w(0)
            for h in range(8):
                wa = ARENA[h % 2][:, :].rearrange("p (k c) -> p k c", k=16)
                bw = B_AR[h % 2]
                if h + 1 < 8:
                    load_head_w(h + 1)
                for half in range(2):
                    tiles = [8 + half * 4 + i for i in range(4)]
                    for k in range(16):
                        mm(PS[2][:, :], wa[:, k, 0:128], uT[:, k, 1024 + half * 512:1024 + (half + 1) * 512], k == 0, k == 15,
                           [bw] + [B_uT[t] for t in tiles], [PB[2]])
                    cp("act", hqT[:, half * 512:(half + 1) * 512], PS[2][:, :], [PB[2]], [B_hqT[half]])
                for t in range(NT):
                    own = t >= 8
                    ncol = 384 if own else 256
                    bank = t % 2
                    for k in range(16):
                        mm(PS[bank][:, 0:ncol], uT[:, k, t * 128:(t + 1) * 128], wa[:, k, 128:128 + ncol], k == 0, k == 15,
                           [bw, B_uT[t]], [PB[bank]])
                    act(kt[:, t, :], PS[bank][:, 0:128], AF.Sigmoid, [PB[bank]], [B_kt[t]], scale=-1.0)
                    tt("pool", kt[:, t, :], kt[:, t, :], omlb[:, h * 128:(h + 1) * 128], ALU.mult, [B_kt[t], B_omlb], [B_kt[t]])
                    if own:
                        cp("act", vv[:, t, :], PS[bank][:, 128:256], [PB[bank]], [B_vv[t]])
                        i2 = t % 2
                        act(sgm[i2][:, :], PS[bank][:, 256:384], AF.Sigmoid, [PB[bank]], [B_sgm[i2]])
                        tt("dve", sg[:, t - 8, :], sgm[i2][:, :], PS[bank][:, 256:384], ALU.mult, [B_sgm[i2], PB[bank]], [B_sg[t - 8]])
                        tt("pool", sg[:, t - 8, :], sg[:, t - 8, :], outg[:, :], ALU.mult, [B_sg[t - 8], B_outg], [B_sg[t - 8]])
                    else:
                        act(vv[:, t, :], PS[bank][:, 128:256], AF.Identity, [PB[bank], B_pm], [B_vv[t]], scale=pm[:, 0:1])
                act(lf[:, :, :], kt[:, :, :], AF.Ln, B_kt + [B_eps], [B_lf], scale=-1.0, bias=one_t[:, 0:1])
                memset("pool", S_f[:, :], 0.0, [B_S])
                memset("pool", S_b[:, :], 0.0, [B_Sb])

                def cum(t):
                    bank = 3 + t % 2
                    own = t >= 8
                    mm(PS[bank][:, 0:128], lf[:, t, :], tri_f[:, :], True, True, [B_lf, B_trif], [PB[bank]])
                    mm(PS[bank][:, 128:256], trs_f[:, :], lf[:, t, :], True, True, [B_lf, B_trsf], [PB[bank]])
                    if own:
                        tr(PS[bank][:, 256:384], kt[:, t, :], ident_f[:, :], [B_kt[t], B_identf], [PB[bank]])
                    i = t % R
                    act(ec[i][:, :], PS[bank][:, 128:256], AF.Exp, [PB[bank]], [B_ec[i]])
                    act(dec[i][:, :], PS[bank][:, 127:128], AF.Exp, [PB[bank]], [B_dec[i]])
                    tt("pool", Kh[i][:, :], kt[:, t, :], ec[i][:, :], ALU.mult, [B_kt[t], B_ec[i]], [B_Kh[i]])
                    if own:
                        act(eb[i][:, :], PS[bank][:, 0:128], AF.Exp, [PB[bank]], [B_eb[i]])
                        act(enb[i][:, :], PS[bank][:, 0:128], AF.Exp, [PB[bank]], [B_enb[i]], scale=-1.0)
                        c0 = (t - 8) * 128
                        tt("pool", Qt[i][:, :], hqT[:, c0:c0 + 128], eb[i][:, :], ALU.mult, [B_hqT[(t - 8) // 4], B_eb[i]], [B_Qt[i]])
                        tt("dve", Kt[i][:, :], PS[bank][:, 256:384], enb[i][:, :], ALU.mult, [PB[bank], B_enb[i]], [B_Kt[i]])

                cum(0)
                for t in range(NT):
                    own = t >= 8
                    i = t % R
                    if t + 1 < NT:
                        cum(t + 1)
                    if own:
                        mm(PS[5][:, 0:128], Kt[i][:, :], Qt[i][:, :], True, True, [B_Kt[i], B_Qt[i]], [PB[5]])
                        tt("dve", ATm[i][:, :], PS[5][:, 0:128], tri_f[:, :], ALU.mult, [PB[5], B_trif], [B_ATm[i]])
                        mm(PS[6][:, 0:128], Qt[i][:, :], S_b[:, :], True, False, [B_Qt[i], B_Sb], [PB[6]])
                        mm(PS[6][:, 0:128], ATm[i][:, :], vv[:, t, :], False, True, [B_ATm[i], B_vv[t]], [PB[6]])
                    mm(PS[7][:, 0:128], Kh[i][:, :], vv[:, t, :], True, True, [B_Kh[i], B_vv[t]], [PB[7]])
                    stt(S_f[:, :], S_f[:, :], dec[i][:, 0:1], PS[7][:, 0:128], ALU.mult, ALU.add, [B_S, B_dec[i], PB[7]], [B_S])
                    cp("act", S_b[:, :], S_f[:, :], [B_S], [B_Sb])
                    if own:
                        act(osq[:, :], PS[6][:, 0:128], AF.Square, [PB[6]], [B_osq, B_o1], accum=oss[:, 0:1])
                        rstd_from_ss(oss[:, 2:3], oss[:, 0:1], 128.0, [B_o1, B_eps], [B_o3], oss[:, 1:2], B_o2)
                        stt(merged[:, t - 8, h * 128:(h + 1) * 128], PS[6][:, 0:128], oss[:, 2:3], sg[:, t - 8, :], ALU.mult, ALU.mult,
                            [PB[6], B_o3, B_sg[t - 8]], [B_mg[t - 8]])
            barrier()

        if stage == 1:
            with ExitStack() as ph:
                dmp = sb(ph, "dmp", [128, NOWN, 1024], F32); B_dmp = Buf("dmp")
                for t in range(NOWN):
                    cp("dve", dmp[:, t, :], merged[:, t, 0:1024], [B_mg[t], B_dmp], [B_dmp])
                load("sp", dbg_d.rearrange("(t p) d -> p t d", p=128)[:, :, 0:1024], dmp[:, :, :], [], B_dmp, reads=[B_dmp])
                P.op("sp", lambda h: h.nop(), [], [B_dmp])
                P.finalize()
            return nc

        SCALE = 192.0 ** -0.5
        with ExitStack() as ph:
            cqnT = sb(ph, "cqnT", [128, 4, 1024], BF16); B_cqnT = [Buf("cqnT%d" % t) for t in range(NOWN)]
            ckvnT = sb(ph, "ckvnT", [128, 2, NT * 128], BF16); B_ckvnT = [Buf("ckvnT%d" % t) for t in range(NT)]
            KRg = sb(ph, "KRg", [128, NT, 64], F32); B_KRg = [Buf("KRg%d" % t) for t in range(NT)]
            ssr = sb(ph, "ssr", [128, NT], F32); B_ssr = [Buf("ssr%d" % t) for t in range(NT)]
            QNGr = sb(ph, "QNGr", [128, 64], F32); B_QNGr = Buf("QNGr")
            KNGr = sb(ph, "KNGr", [128, 64], F32); B_KNGr = Buf("KNGr")
            MOG = sb(ph, "MOG", [128, 1024], F32); B_MOG = Buf("MOG")
            load("sp", QNGr[:, :], qng_d[128:192].partition_broadcast(128), [B_QNGr], B_QNGr)
            load("sp", KNGr[:, :], kng_d[128:192].partition_broadcast(128), [B_KNGr], B_KNGr)
            load("sp", MOG[:, :], mog_d.partition_broadcast(128), [B_MOG], B_MOG)
            sm = sb(ph, "sm", [128, 64], F32)
            B_sm = [Buf("sm%d" % i) for i in range(64)]
            ckvn = [sb(ph, "ckvn%d" % i, [128, 256], BF16) for i in range(2)]; B_ckvn = [Buf("ckvn0"), Buf("ckvn1")]
            cqn = [sb(ph, "cqn%d" % i, [128, 512], BF16) for i in range(2)]; B_cqn = [Buf("cqn0"), Buf("cqn1")]
            junk = sb(ph, "junk", [128, 512], BF16); B_junk = Buf("junk")
            xg = [sb(ph, "xg%d" % i, [128, 64], F32) for i in range(2)]; B_xg = [Buf("xg0"), Buf("xg1")]
            rt = [sb(ph, "rt%d" % i, [128, 4, 32], F32) for i in range(2)]; B_rt = [Buf("rt0"), Buf("rt1")]

            def rope(eng, dst1, dst2, x, cosv, sinv, tmp, reads, B_tmp, writes):
                tt(eng, tmp[:, 0, :], x[:, 0:32], cosv, ALU.mult, reads, [B_tmp])
                tt(eng, tmp[:, 1, :], x[:, 32:64], sinv, ALU.mult, reads, [B_tmp])
                tt(eng, tmp[:, 2, :], x[:, 32:64], cosv, ALU.mult, reads, [B_tmp])
                tt(eng, tmp[:, 3, :], x[:, 0:32], sinv, ALU.mult, reads, [B_tmp])
                tt(eng, dst1, tmp[:, 0, :], tmp[:, 1, :], ALU.subtract, [B_tmp], writes)
                tt(eng, dst2, tmp[:, 2, :], tmp[:, 3, :], ALU.add, [B_tmp], writes)

            A0 = ARENA[0][:, :].rearrange("p (k c) -> p k c", k=16)
            A1 = ARENA[1][:, 0:16 * 320].rearrange("p (k c) -> p k c", k=16)
            P.dma("pool", lambda hh: hh.dma_start(out=A0, in_=w3[:, :, 4096:4608]), [], [B_AR[0]], sembuf=B_AR[0])
            P.dma("pool", lambda hh: hh.dma_start(out=A1, in_=w3[:, :, 4608:4928]), [], [B_AR[1]], sembuf=B_AR[1])
            for t in range(NT):
                own = t >= 8
                i2 = t % 2
                bk = 2 + i2
                for k in range(16):
                    mm(PS[bk][:, 0:320], uT[:, k, t * 128:(t + 1) * 128], A1[:, k, :], k == 0, k == 15, [B_AR[1], B_uT[t]], [PB[bk]])
                s0, s1, s2 = 0 + 8 * i2, 1 + 8 * i2, 2 + 8 * i2
                act(junk[:, 0:256], PS[bk][:, 0:256], AF.Square, [PB[bk]], [B_junk, B_sm[s0]], accum=sm[:, s0:s0 + 1])
                rstd_from_ss(sm[:, s2:s2 + 1], sm[:, s0:s0 + 1], 256.0, [B_sm[s0], B_eps], [B_sm[s2]], sm[:, s1:s1 + 1], B_sm[s1])
                ts("dve", ckvn[i2][:, :], PS[bk][:, 0:256], sm[:, s2:s2 + 1], None, ALU.mult, None, [PB[bk], B_sm[s2]], [B_ckvn[i2]])
                act(junk[:, 256:320], PS[bk][:, 256:320], AF.Square, [PB[bk]], [B_junk, B_ssr[t]], accum=ssr[:, t:t + 1])
                tt("dve", xg[i2][:, :], PS[bk][:, 256:320], KNGr[:, :], ALU.mult, [PB[bk], B_KNGr], [B_xg[i2]])
                rope("pool", KRg[:, t, 0:32], KRg[:, t, 32:64], xg[i2], cosT[:, t, :], sinT[:, t, :], rt[i2],
                     [B_xg[i2], B_cos, B_sin], B_rt[i2], [B_KRg[t]])
                tb = 4 + i2
                pv = psbf(tb)
                for c in range(2):
                    tr(pv[:, c * 128:(c + 1) * 128], ckvn[i2][:, c * 128:(c + 1) * 128], ident_b[:, :], [B_ckvn[i2], B_identb], [PB[tb]])
                for c in range(2):
                    ts("dve", ckvnT[:, c, t * 128:(t + 1) * 128], pv[:, c * 128:(c + 1) * 128], colA[:, 52 + c:53 + c], None, ALU.mult, None,
                       [PB[tb], B_colA], [B_ckvnT[t]])
                if own:
                    qb = i2
                    for k in range(16):
                        mm(PS[qb][:, :], uT[:, k, t * 128:(t + 1) * 128], A0[:, k, :], k == 0, k == 15, [B_AR[0], B_uT[t]], [PB[qb]])
                    s3, s4, s5 = 3 + 8 * i2, 4 + 8 * i2, 5 + 8 * i2
                    act(junk[:, :], PS[qb][:, :], AF.Square, [PB[qb]], [B_junk, B_sm[s3]], accum=sm[:, s3:s3 + 1])
                    rstd_from_ss(sm[:, s5:s5 + 1], sm[:, s3:s3 + 1], 512.0, [B_sm[s3], B_eps], [B_sm[s5]], sm[:, s4:s4 + 1], B_sm[s4])
                    ts("dve", cqn[i2][:, :], PS[qb][:, :], sm[:, s5:s5 + 1], None, ALU.mult, None, [PB[qb], B_sm[s5]], [B_cqn[i2]])
                    tb2 = 6 + i2
                    pv2 = psbf(tb2)
                    for c in range(4):
                        tr(pv2[:, c * 128:(c + 1) * 128], cqn[i2][:, c * 128:(c + 1) * 128], ident_b[:, :], [B_cqn[i2], B_identb], [PB[tb2]])
                    for c in range(4):
                        o = cqnT[:, c, (t - 8) * 128:(t - 7) * 128]
                        if t % 2 == 0:
                            act(o, pv2[:, c * 128:(c + 1) * 128], AF.Identity, [PB[tb2], B_colA], [B_cqnT[t - 8]], scale=colA[:, 48 + c:49 + c])
                        else:
                            ts("dve", o, pv2[:, c * 128:(c + 1) * 128], colA[:, 48 + c:49 + c], None, ALU.mult, None, [PB[tb2], B_colA], [B_cqnT[t - 8]])

            if DBG_SUB == "A":
                P.finalize()
                return nc
            Wkv = ARENA[0][:, 0:4096].rearrange("p (c n) -> p c n", c=2)
            Wuq = ARENA[1][:, 0:6144].rearrange("p (c n) -> p c n", c=4)
            for c in range(2):
                P.dma("pool", (lambda c=c: (lambda hh: hh.dma_start(out=Wkv[:, c, :], in_=w_ukv_d[c * 128:(c + 1) * 128, :], max_dma_last_dim=4096)))(),
                      [], [B_AR[0]], sembuf=B_AR[0])
            for c in range(4):
                P.dma("pool", (lambda c=c: (lambda hh: hh.dma_start(out=Wuq[:, c, :], in_=w_uq_d[c * 128:(c + 1) * 128, :], max_dma_last_dim=3072)))(),
                      [], [B_AR[1]], sembuf=B_AR[1])
            KTn = sb(ph, "KTn", [128, NT * 128], BF16); B_KTn = [Buf("KTn%d" % t) for t in range(NT)]
            KTr = sb(ph, "KTr", [128, NT * 128], BF16); B_KTr = [Buf("KTr%d" % t) for t in range(NT)]
            VA = sb(ph, "VA", [128, NT, 132], BF16); B_VA = [Buf("VA%d" % t) for t in range(NT)]
            QTn = sb(ph, "QTn", [128, 1024], BF16); B_QTn = [Buf("QTn%d" % t) for t in range(NOWN)]
            QTr = sb(ph, "QTr", [128, 1024], BF16); B_QTr = [Buf("QTr%d" % t) for t in range(NOWN)]
            PT = [sb(ph, "PT%d" % i, [128, 512], BF16) for i in range(4)]; B_PT = [Buf("PT%d" % i) for i in range(4)]
            B_zpad = Buf("zpad")
            memset("pool", KTr[64:128, :], 0.0, [B_zpad])
            memset("pool", QTr[64:128, :], 0.0, [B_zpad])
            kn = [sb(ph, "kn%d" % i, [128, 128], BF16) for i in range(4)]; B_kn = [Buf("kn%d" % i) for i in range(4)]
            krn = [sb(ph, "krn%d" % i, [128, 64], BF16) for i in range(4)]; B_krn = [Buf("krn%d" % i) for i in range(4)]
            qn = [sb(ph, "qn%d" % i, [128, 128], BF16) for i in range(4)]; B_qn = [Buf("qn%d" % i) for i in range(4)]
            qr = [sb(ph, "qr%d" % i, [128, 64], BF16) for i in range(4)]; B_qr = [Buf("qr%d" % i) for i in range(4)]
            xq = [sb(ph, "xq%d" % i, [128, 64], F32) for i in range(4)]; B_xq = [Buf("xq%d" % i) for i in range(4)]
            rq = [sb(ph, "rq%d" % i, [128, 4, 32], F32) for i in range(4)]; B_rq = [Buf("rq%d" % i) for i in range(4)]
            KVB = [0, 1, 5, 6]
            TRB = [2, 7]
            tri_ = 0
            ssh = sb(ph, "ssh", [128, NOWN, 8], F32); B_ssh = [Buf("ssh%d" % t) for t in range(NOWN)]
            rden = sb(ph, "rden", [128, NOWN], F32); B_rden = [Buf("rden%d" % t) for t in range(NOWN)]
            for t in range(NT):
                if t >= 8:
                    memset("pool", VA[:, t, 128:129], 1.0, [B_VA[t]])
                else:
                    cp("pool", VA[:, t, 128:129], pm[:, 0:1], [B_pm], [B_VA[t]])
            pt_i = 0
            if DBG_SUB == "B0":
                P.finalize()
                return nc
            for h in range(8):
                for t in range(NT):
                    i2 = t % 4
                    bk = KVB[t % 4]
                    for c in range(2):
                        mm(PS[bk][:, 0:256], ckvnT[:, c, t * 128:(t + 1) * 128], Wkv[:, c, h * 256:(h + 1) * 256], c == 0, c == 1,
                           [B_AR[0], B_ckvnT[t]], [PB[bk]])
                    s0, s1, s2 = 16 + 4 * i2, 17 + 4 * i2, 18 + 4 * i2
                    act(junk[:, 0:128], PS[bk][:, 0:128], AF.Square, [PB[bk]], [B_junk, B_sm[s0]], accum=sm[:, s0:s0 + 1])
                    tt("dve", sm[:, s0:s0 + 1], sm[:, s0:s0 + 1], ssr[:, t:t + 1], ALU.add, [B_sm[s0], B_ssr[t]], [B_sm[s0]])
                    rstd_from_ss(sm[:, s2:s2 + 1], sm[:, s0:s0 + 1], 192.0, [B_sm[s0], B_eps], [B_sm[s2]], sm[:, s1:s1 + 1], B_sm[s1])
                    ts("dve", kn[i2][:, :], PS[bk][:, 0:128], sm[:, s2:s2 + 1], None, ALU.mult, None, [PB[bk], B_sm[s2]], [B_kn[i2]])
                    ts("pool", krn[i2][:, :], KRg[:, t, :], sm[:, s2:s2 + 1], None, ALU.mult, None, [B_KRg[t], B_sm[s2]], [B_krn[i2]])
                    if t >= 8:
                        cp("act", VA[:, t, 0:128], PS[bk][:, 128:256], [PB[bk]], [B_VA[t]])
                    else:
                        act(VA[:, t, 0:128], PS[bk][:, 128:256], AF.Identity, [PB[bk], B_pm], [B_VA[t]], scale=pm[:, 0:1])
                    tb = TRB[tri_ % 2]
                    tri_ += 1
                    pv = psbf(tb)
                    tr(pv[:, 0:128], kn[i2][:, :], ident_b[:, :], [B_kn[i2], B_identb], [PB[tb]])
                    tr(pv[0:64, 128:256], krn[i2][:, :], ident_b[:, :], [B_krn[i2], B_identb], [PB[tb]])
                    ts("dve", KTn[:, t * 128:(t + 1) * 128], pv[:, 0:128], colA[:, 56:57], None, ALU.mult, None, [PB[tb], B_colA], [B_KTn[t]])
                    cp("act", KTr[0:64, t * 128:(t + 1) * 128], pv[0:64, 128:256], [PB[tb]], [B_KTr[t]])
                if DBG_SUB == "B1k":
                    P.finalize()
                    return nc
                for t in range(NOWN):
                    i2 = t % 4
                    qb = 3 + t % 2
                    for c in range(4):
                        mm(PS[qb][:, 0:192], cqnT[:, c, t * 128:(t + 1) * 128], Wuq[:, c, h * 192:(h + 1) * 192], c == 0, c == 3,
                           [B_AR[1], B_cqnT[t]], [PB[qb]])
                    s0, s1, s2 = 32 + 4 * i2, 33 + 4 * i2, 34 + 4 * i2
                    act(junk[:, 0:192], PS[qb][:, 0:192], AF.Square, [PB[qb]], [B_junk, B_sm[s0]], accum=sm[:, s0:s0 + 1])
                    rstd_from_ss(sm[:, s2:s2 + 1], sm[:, s0:s0 + 1], 192.0, [B_sm[s0], B_eps], [B_sm[s2]], sm[:, s1:s1 + 1], B_sm[s1])
                    ts("dve", qn[i2][:, :], PS[qb][:, 0:128], sm[:, s2:s2 + 1], None, ALU.mult, None, [PB[qb], B_sm[s2]], [B_qn[i2]])
                    stt(xq[i2][:, :], PS[qb][:, 128:192], sm[:, s2:s2 + 1], QNGr[:, :], ALU.mult, ALU.mult, [PB[qb], B_sm[s2], B_QNGr], [B_xq[i2]])
                    rope("pool", qr[i2][:, 0:32], qr[i2][:, 32:64], xq[i2], cosT[:, 8 + t, :], sinT[:, 8 + t, :], rq[i2],
                         [B_xq[i2], B_cos, B_sin], B_rq[i2], [B_qr[i2]])
                    tb = TRB[tri_ % 2]
                    tri_ += 1
                    pv = psbf(tb)
                    tr(pv[:, 256:384], qn[i2][:, :], ident_b[:, :], [B_qn[i2], B_identb], [PB[tb]])
                    tr(pv[0:64, 384:512], qr[i2][:, :], ident_b[:, :], [B_qr[i2], B_identb], [PB[tb]])
                    ts("dve", QTn[:, t * 128:(t + 1) * 128], pv[:, 256:384], colA[:, 54:55], None, ALU.mult, None, [PB[tb], B_colA], [B_QTn[t]])
                    cp("act", QTr[0:64, t * 128:(t + 1) * 128], pv[0:64, 384:512], [PB[tb]], [B_QTr[t]])
                if DBG_SUB == "B1":
                    P.finalize()
                    return nc
                for bnk in (5, 6, 7):
                    P.op("dve", (lambda bnk=bnk: (lambda hh: hh.memset(PS[bnk][:, 0:387], 0.0)))(), [], [PB[bnk]])

                def Oap(i):
                    return PS[5 + i // 3][:, (i % 3) * 129:(i % 3) * 129 + 129]

                sbank = 0
                for j in range(NT):
                    qs = 0 if j < 8 else j - 8
                    c0 = qs * 128
                    while c0 < 1024:
                        n = min(512, 1024 - c0)
                        bk = 3 + sbank % 2
                        sbank += 1
                        qtiles = list(range(c0 // 128, (c0 + n) // 128))
                        mm(PS[bk][:, 0:n], KTn[:, j * 128:(j + 1) * 128], QTn[:, c0:c0 + n], True, False,
                           [B_KTn[j]] + [B_QTn[i] for i in qtiles], [PB[bk]])
                        mm(PS[bk][:, 0:n], KTr[:, j * 128:(j + 1) * 128], QTr[:, c0:c0 + n], False, True,
                           [B_KTr[j], B_zpad] + [B_QTr[i] for i in qtiles], [PB[bk]])
                        r = pt_i % 4
                        pt_i += 1
                        act(PT[r][:, 0:n], PS[bk][:, 0:n], AF.Exp, [PB[bk], B_eps], [B_PT[r]], scale=SCALE, bias=shift_t[:, 0:1])
                        if j >= 8 and c0 == qs * 128:
                            tt("pool", PT[r][:, 0:128], PT[r][:, 0:128], tri_b[:, :], ALU.mult, [B_PT[r], B_trib], [B_PT[r]])
                        for i in qtiles:
                            ob = 5 + i // 3
                            mm(Oap(i), PT[r][:, i * 128 - c0:i * 128 - c0 + 128], VA[:, j, 0:129], False, False,
                               [B_PT[r], B_VA[j]], [PB[ob]], skip=True)
                        c0 += n
                if DBG_SUB == "B2":
                    P.finalize()
                    return nc
                for i in range(NOWN):
                    ob = 5 + i // 3
                    O = Oap(i)
                    P.op("dve", (lambda i=i, O=O: (lambda hh: hh.reciprocal(out=rden[:, i:i + 1], in_=O[:, 128:129])))(), [PB[ob]], [B_rden[i]])
                    ts("dve", merged[:, i, 1024 + h * 128:1024 + (h + 1) * 128], O[:, 0:128], rden[:, i:i + 1], None, ALU.mult, None,
                       [PB[ob], B_rden[i]], [B_mg[i]])
                    act(junk[:, 0:128], O[:, 0:128], AF.Square, [PB[ob], B_rden[i]], [B_junk, B_ssh[i]], scale=rden[:, i:i + 1],
                        accum=ssh[:, i, h:h + 1])
            for i in range(NOWN):
                s0, s1, s2 = 48, 49, 50
                P.op("dve", (lambda i=i: (lambda hh: hh.tensor_reduce(out=sm[:, 48:49], in_=ssh[:, i, :], axis=AX.X, op=ALU.add)))(),
                     [B_ssh[i]], [B_sm[s0]])
                rstd_from_ss(sm[:, s2:s2 + 1], sm[:, s0:s0 + 1], 1024.0, [B_sm[s0], B_eps], [B_sm[s2]], sm[:, s1:s1 + 1], B_sm[s1])
                stt(merged[:, i, 1024:2048], merged[:, i, 1024:2048], sm[:, s2:s2 + 1], MOG[:, :], ALU.mult, ALU.mult,
                    [B_mg[i], B_sm[s2], B_MOG], [B_mg[i]])
            barrier()

        mx.close()

        if stage == 2:
            with ExitStack() as ph:
                dmp = sb(ph, "dmp", [128, NOWN, D], F32); B_dmp = Buf("dmp")
                for t in range(NOWN):
                    cp("dve", dmp[:, t, :], merged[:, t, :], [B_mg[t], B_dmp], [B_dmp])
                load("sp", dbg_d.rearrange("(t p) d -> p t d", p=128), dmp[:, :, :], [], B_dmp, reads=[B_dmp])
                P.op("sp", lambda h: h.nop(), [], [B_dmp])
                P.finalize()
            return nc

        hres = sb(st, "hres", [128, NOWN, D], F32); B_hres = [Buf("hres%d" % t) for t in range(NOWN)]
        GT = sb(st, "GT", [128, D], F32); B_GT = Buf("GT")
        Dg = [sb(st, "Dg%d" % i, [128, 128], F32) for i in range(2)]; B_Dg = [Buf("Dg0"), Buf("Dg1")]

        def row_broadcast(col_ap, B_col, bank0=0):
            for c in range(16):
                i2 = c % 2
                ts("dve", Dg[i2][:, :], ident_f[:, :], col_ap[:, c:c + 1], None, ALU.mult, None, [B_identf, B_col], [B_Dg[i2]])
                bk = bank0 + i2
                mm(PS[bk][:, 0:128], ones_f[:, :], Dg[i2][:, :], True, True, [B_ones, B_Dg[i2]], [PB[bk]])
                cp("act", GT[:, c * 128:(c + 1) * 128], PS[bk][:, 0:128], [PB[bk]], [B_GT])

        with ExitStack() as ph:
            mT = sb(ph, "mT", [128, 16, 1024], BF16); B_mT = [Buf("mT%d" % t) for t in range(NOWN)]
            xs = [sb(ph, "xs%d" % i, [128, 512], F32) for i in range(3)]; B_xs = [Buf("xs%d" % i) for i in range(3)]
            row_broadcast(modT[:, 32:48], B_mod2)
            for i in range(NOWN):
                for half in range(2):
                    bank = 4 + (2 * i + half) % 4
                    pv = psbf(bank)
                    for c8 in range(8):
                        c = half * 8 + c8
                        tr(pv[:, c8 * 128:(c8 + 1) * 128], merged[:, i, c * 128:(c + 1) * 128], ident_b[:, :], [B_mg[i], B_identb], [PB[bank]])
                    o = mT[:, half * 8:(half + 1) * 8, i * 128:(i + 1) * 128]
                    src = pv[:, :].rearrange("p (c x) -> p c x", c=8)
                    if half == 0:
                        cp("act", o, src, [PB[bank]], [B_mT[i]])
                    else:
                        cp("dve", o, src, [PB[bank]], [B_mT[i]])
            wo3 = w_out_d.rearrange("(k p) c -> p k c", p=128)
            xi = 0
            for n in range(4):
                Wo = ARENA[n % 2][:, :].rearrange("p (k c) -> p k c", k=16)
                bw = B_AR[n % 2]
                P.dma("pool", (lambda n=n, Wo=Wo: (lambda hh: hh.dma_start(out=Wo, in_=wo3[:, :, n * 512:(n + 1) * 512])))(), [], [bw], sembuf=bw)
                gtb = GT[:, n * 512:(n + 1) * 512].unsqueeze(1).to_broadcast([128, 16, 512])
                tt("pool", Wo, Wo, gtb, ALU.mult, [bw, B_GT], [bw])
                for i in range(NOWN):
                    x_ = xs[xi % 3]
                    Bx = B_xs[xi % 3]
                    xi += 1
                    load("sp", x_[:, :], xo_d[i * 128:(i + 1) * 128, n * 512:(n + 1) * 512], [Bx], Bx)
                    bk = (n * NOWN + i) % 4
                    for k in range(16):
                        mm(PS[bk][:, :], mT[:, k, i * 128:(i + 1) * 128], Wo[:, k, :], k == 0, k == 15, [bw, B_mT[i]], [PB[bk]])
                    tt("dve", hres[:, i, n * 512:(n + 1) * 512], PS[bk][:, :], x_[:, :], ALU.add, [PB[bk], Bx], [B_hres[i]])
            barrier()

        if stage == 3:
            load("sp", dbg_d.rearrange("(t p) d -> p t d", p=128), hres[:, :, :], [], B_hres[0], reads=B_hres)
            P.op("sp", lambda h: h.nop(), [], B_hres)
            P.finalize()
            return nc

        with ExitStack() as ph:
            u2T = sb(ph, "u2T", [128, 16, 1024], BF16); B_u2T = [Buf("u2T%d" % t) for t in range(NOWN)]
            wg = sb(ph, "wg", [128, NOWN, 64], F32); B_wg = [Buf("wg%d" % t) for t in range(NOWN)]
            row_broadcast(modT[:, 80:96], B_mod2, bank0=2)
            UN = [ARENA[0][:, :], ARENA[1][:, :],
                  merged[:, 0:4, :].rearrange("p a d -> p (a d)"), merged[:, 4:8, :].rearrange("p a d -> p (a d)")]
            B_UN = [B_AR[0], B_AR[1], Buf("un2"), Buf("un3")]
            gtb4 = GT[:, :].unsqueeze(1).to_broadcast([128, 4, D])
            n_tot = n_exp + 1

            def wsrc(e):
                if e < n_exp:
                    return w_g_d[e], w_u_d[e], w_d_d[e]
                return ws_g_d, ws_u_d, ws_d_d

            def issue_loads(e, which):
                g_d, u_d, d_d = wsrc(e)
                for i, src in ((0, g_d), (1, u_d)):
                    if i not in which:
                        continue
                    u = (3 * e + i) % 4
                    dst = UN[u].rearrange("p (k c) -> p k c", k=16)
                    P.dma("pool", (lambda dst=dst, src=src: (lambda hh: hh.dma_start(out=dst, in_=src.rearrange("(k p) c -> p k c", p=128))))(),
                          [], [B_UN[u]], sembuf=B_UN[u])
                if 2 not in which:
                    return
                u = (3 * e + 2) % 4
                dst = UN[u].rearrange("p (c n) -> p c n", c=4)
                for c in range(4):
                    P.dma("pool", (lambda dst=dst, c=c, d_d=d_d: (lambda hh: hh.dma_start(out=dst[:, c, :], in_=d_d[c * 128:(c + 1) * 128, :],
                                                                                           max_dma_last_dim=4096)))(),
                          [], [B_UN[u]], sembuf=B_UN[u])
                tt("pool", dst, dst, gtb4, ALU.mult, [B_UN[u], B_GT], [B_UN[u]])

            issue_loads(0, (0, 1, 2))
            with ExitStack() as ph2:
                norm_to_T(ph2, lambda t: (hres[:, t, :], B_hres[t]), NOWN, u2T, B_u2T, af_, modT[:, 48:64], B_af, B_mod2, "n2")
                Wr = sb(ph2, "Wr", [128, 16, 64], BF16); B_Wr = Buf("Wr")
                RB = sb(ph2, "RB", [128, 64], F32); B_RB = Buf("RB")
                P.dma("pool", lambda hh: hh.dma_start(out=Wr[:, :, :], in_=w_r_d.rearrange("(k p) e -> p k e", p=128)), [], [B_Wr], sembuf=B_Wr)
                load("sp", RB[:, :], rb_d.partition_broadcast(128), [B_RB], B_RB)
                sc = sb(ph2, "sc", [128, 64], F32); B_sc = Buf("sc")
                sel = sb(ph2, "sel", [128, 64], F32); B_sel = Buf("sel")
                selm = sb(ph2, "selm", [128, 64], F32); B_selm = Buf("selm")
                m8 = sb(ph2, "m8", [128, 8, 8], F32); B_m8 = Buf("m8")
                gs = sb(ph2, "gs", [128, 8], F32); B_gs = Buf("gs")
                gm8 = sb(ph2, "gm8", [128, 8], F32); B_gm8 = Buf("gm8")
                gmk = sb(ph2, "gmk", [128, 8], F32); B_gmk = Buf("gmk")
                pen = sb(ph2, "pen", [128, 8], F32); B_pen = Buf("pen")
                em8 = sb(ph2, "em8", [128, 8], F32); B_em8 = Buf("em8")
                emk = sb(ph2, "emk", [128, 64], F32); B_emk = Buf("emk")
                den = sb(ph2, "den", [128, 2], F32); B_den = Buf("den")
                for t in range(NOWN):
                    bk = t % 2
                    for k in range(16):
                        mm(PS[bk][:, 0:64], u2T[:, k, t * 128:(t + 1) * 128], Wr[:, k, :], k == 0, k == 15, [B_Wr, B_u2T[t]], [PB[bk]])
                    act(sc[:, :], PS[bk][:, 0:64], AF.Sigmoid, [PB[bk]], [B_sc])
                    tt("dve", sel[:, :], sc[:, :], RB[:, :], ALU.add, [B_sc, B_RB], [B_sel])
                    for g in range(8):
                        P.op("dve", (lambda g=g: (lambda hh: hh.max(out=m8[:, g, :], in_=sel[:, g * 8:(g + 1) * 8])))(), [B_sel], [B_m8])
                    tt("dve", gs[:, :], m8[:, :, 0], m8[:, :, 1], ALU.add, [B_m8], [B_gs])
                    P.op("dve", lambda hh: hh.max(out=gm8[:, :], in_=gs[:, :]), [B_gs], [B_gm8])
                    ts("dve", gmk[:, :], gs[:, :], gm8[:, 3:4], None, ALU.is_ge, None, [B_gs, B_gm8], [B_gmk])
                    ts("dve", pen[:, :], gmk[:, :], 4.0, -4.0, ALU.mult, ALU.add, [B_gmk], [B_pen])
                    sel3 = sel[:, :].rearrange("p (g e) -> p g e", g=8)
                    selm3 = selm[:, :].rearrange("p (g e) -> p g e", g=8)
                    tt("dve", selm3, sel3, gmk[:, :].unsqueeze(2).to_broadcast([128, 8, 8]), ALU.mult, [B_sel, B_gmk], [B_selm])
                    tt("dve", selm3, selm3, pen[:, :].unsqueeze(2).to_broadcast([128, 8, 8]), ALU.add, [B_selm, B_pen], [B_selm])
                    P.op("dve", lambda hh: hh.max(out=em8[:, :], in_=selm[:, :]), [B_selm], [B_em8])
                    ts("dve", emk[:, :], selm[:, :], em8[:, 7:8], None, ALU.is_ge, None, [B_selm, B_em8], [B_emk])
                    tt("dve", emk[:, :], emk[:, :], sc[:, :], ALU.mult, [B_emk, B_sc], [B_emk])
                    P.op("dve", lambda hh: hh.tensor_reduce(out=den[:, 0:1], in_=emk[:, :], axis=AX.X, op=ALU.add), [B_emk], [B_den])
                    ts("dve", den[:, 0:1], den[:, 0:1], 0.4, None, ALU.mult, None, [B_den], [B_den])
                    P.op("dve", lambda hh: hh.reciprocal(out=den[:, 1:2], in_=den[:, 0:1]), [B_den], [B_den])
                    ts("dve", wg[:, t, :], emk[:, :], den[:, 1:2], None, ALU.mult, None, [B_emk, B_den], [B_wg[t]])
                barrier()
                if DBG_SUB == "WG":
                    dbg2_d = nc.dram_tensor("dbg2", [128, 512], F32, kind="ExternalOutput").ap()
                    load("sp", dbg2_d[:, :], wg[:, :, :].rearrange("p t e -> p (t e)"), [], B_wg[0], reads=B_wg)
                    P.op("sp", lambda h: h.nop(), [], B_wg)

            hid = sb(ph, "hid", [128, 4, 1024], BF16); B_hid = [Buf("hidA"), Buf("hidB")]
            sgl = [sb(ph, "sgl%d" % i, [128, 512], F32) for i in range(2)]; B_sgl = [Buf("sgl0"), Buf("sgl1")]
            dbi = 0
            for e in range(n_tot):
                if e + 1 < n_tot:
                    issue_loads(e + 1, (0,))
                Wg_ = UN[(3 * e) % 4].rearrange("p (k c) -> p k c", k=16); Bg = B_UN[(3 * e) % 4]
                Wu_ = UN[(3 * e + 1) % 4].rearrange("p (k c) -> p k c", k=16); Bu = B_UN[(3 * e + 1) % 4]
                Wd_ = UN[(3 * e + 2) % 4].rearrange("p (c n) -> p c n", c=4); Bd = B_UN[(3 * e + 2) % 4]
                gi = 0
                for half in range(2):
                    tl = [B_u2T[half * 4 + i] for i in range(4)]
                    for hc in range(4):
                        gb = gi % 2
                        ub = 2 + gi % 2
                        gi += 1
                        for k in range(16):
                            mm(PS[gb][:, :], Wg_[:, k, hc * 128:(hc + 1) * 128], u2T[:, k, half * 512:(half + 1) * 512], k == 0, k == 15,
                               [Bg] + tl, [PB[gb]])
                        for k in range(16):
                            mm(PS[ub][:, :], Wu_[:, k, hc * 128:(hc + 1) * 128], u2T[:, k, half * 512:(half + 1) * 512], k == 0, k == 15,
                               [Bu] + tl, [PB[ub]])
                        act(sgl[gb][:, :], PS[gb][:, :], AF.Silu, [PB[gb]], [B_sgl[gb]])
                        tt("dve", hid[:, hc, half * 512:(half + 1) * 512], sgl[gb][:, :], PS[ub][:, :], ALU.mult, [B_sgl[gb], PB[ub]], [B_hid[half]])
                if e + 1 < n_tot:
                    issue_loads(e + 1, (1, 2))
                for t in range(NOWN):
                    for dg in range(4):
                        db = 4 + dbi % 4
                        dbi += 1
                        for hc in range(4):
                            mm(PS[db][:, :], hid[:, hc, t * 128:(t + 1) * 128], Wd_[:, hc, dg * 512:(dg + 1) * 512], hc == 0, hc == 3,
                               [B_hid[t // 4], Bd], [PB[db]])
                        hsl = hres[:, t, dg * 512:(dg + 1) * 512]
                        if e < n_exp:
                            stt(hsl, PS[db][:, :], wg[:, t, e:e + 1], hsl, ALU.mult, ALU.add, [PB[db], B_wg[t], B_hres[t]], [B_hres[t]])
                        else:
                            tt("dve", hsl, PS[db][:, :], hsl, ALU.add, [PB[db], B_hres[t]], [B_hres[t]])
            for t in range(NOWN):
                load("sp", out_d[t * 128:(t + 1) * 128, :], hres[:, t, :], [], B_hres[t], reads=[B_hres[t]])
            P.op("sp", lambda h: h.nop(), [], B_hres)
            P.finalize()
    return nc


def make_in_maps(inputs, stage=99):
    x = np.ascontiguousarray(inputs["x"], dtype=np.float32)
    c = np.asarray(inputs["c"], dtype=np.float32)
    pos = np.asarray(inputs["positions"], dtype=np.int32)
    maps = []
    zeros_x = np.zeros((1024, D), np.float32)
    for cid in range(8):
        b, s = cid // 2, cid % 2
        vecA = np.zeros((128, 128), np.float32)
        vecA[0:16] = c[b].reshape(16, 128)
        vecA[16:32] = np.asarray(inputs["norm_mix_g"], np.float32).reshape(16, 128)
        vecA[32:48] = np.asarray(inputs["norm_ffn_g"], np.float32).reshape(16, 128)
        vecA[48:52] = np.asarray(inputs["mla_q_a_g"], np.float32).reshape(4, 128)
        vecA[52:54] = np.asarray(inputs["mla_kv_a_g"], np.float32).reshape(2, 128)
        qn = np.asarray(inputs["mla_q_norm_g"], np.float32).reshape(192)
        kn = np.asarray(inputs["mla_k_norm_g"], np.float32).reshape(192)
        vecA[54, :] = qn[0:128]
        vecA[55, 0:64] = qn[128:192]
        vecA[56, :] = kn[0:128]
        vecA[57, 0:64] = kn[128:192]
        p16 = np.zeros((NT, 128), np.int32)
        if s == 1:
            p16[0:8] = pos[b, 0:1024].reshape(8, 128)
            p16[8:16] = pos[b, 1024:2048].reshape(8, 128)
        else:
            p16[8:16] = pos[b, 0:1024].reshape(8, 128)
        m = {
            "xp": x[b, 0:1024] if s == 1 else zeros_x,
            "xo": x[b, s * 1024:(s + 1) * 1024],
            "vecA": vecA,
            "vecB": np.asarray(inputs["b_ada"], np.float32).reshape(96, 128),
            "pos": np.ascontiguousarray(p16.T),
            "pmask": np.full((128, 1), float(s), np.float32),
            "w_ada": np.asarray(inputs["w_ada"], np.float32).reshape(D, 6 * D),
            "w_in": np.asarray(inputs["w_in"], np.float32).reshape(D, IN_COLS),
            "hg_lb_logits": np.asarray(inputs["hg_lb_logits"], np.float32),
            "hg_out_g": np.asarray(inputs["hg_out_g"], np.float32).reshape(128),
            "mla_w_uq": np.asarray(inputs["mla_w_uq"], np.float32).reshape(512, 1536),
            "mla_w_ukv": np.asarray(inputs["mla_w_ukv"], np.float32).reshape(256, 2048),
            "mla_q_norm_g": qn,
            "mla_k_norm_g": kn,
            "mla_out_g": np.asarray(inputs["mla_out_g"], np.float32).reshape(1024),
            "w_out": np.asarray(inputs["w_out"], np.float32).reshape(D, D),
        }
        if stage >= 4:
            m.update({
                "w_router": np.asarray(inputs["w_router"], np.float32).reshape(D, 64),
                "router_bias": np.asarray(inputs["router_bias"], np.float32).reshape(64),
                "w_gate": np.asarray(inputs["w_gate"], np.float32).reshape(N_EXP, D, 512),
                "w_up": np.asarray(inputs["w_up"], np.float32).reshape(N_EXP, D, 512),
                "w_down": np.asarray(inputs["w_down"], np.float32).reshape(N_EXP, 512, D),
                "ws_gate": np.asarray(inputs["ws_gate"], np.float32).reshape(D, 512),
                "ws_up": np.asarray(inputs["ws_up"], np.float32).reshape(D, 512),
                "ws_down": np.asarray(inputs["ws_down"], np.float32).reshape(512, D),
            })
        maps.append(m)
    return maps


def kernel(**inputs):
    nc = build_program(stage=99)
    in_maps = make_in_maps(inputs, stage=99)
    res = run_bass_kernel_spmd(nc, in_maps, core_ids=list(range(8)))
    out = np.empty((4, S, D), np.float32)
    for cid in range(8):
        b, s = cid // 2, cid % 2
        out[b, s * 1024:(s + 1) * 1024] = res.results[cid]["out"]
    return out
```
